# Optimizing a Trainium2 kernel written in Bass

```python
import jax, jax.numpy as jnp
from jax import lax
import numpy as np

D_MODEL = 1024
BATCH = 1
SEQ = 16384
DEPTH = 2

GRID_W = 64
CTX_LEN = 256
RET_HEADS = 4
RET_DK = 128
RET_DV = 128
RET_CHUNK = 128
WIN_HEADS = 8
WIN_KV_HEADS = 2
WIN_HEAD_DIM = 64
WINDOW = 128
WIN_BLOCK = 128
NA_HEADS = 8
NA_HEAD_DIM = 64
NA_ROWS_MAX = 8
NA_COLS = 16
N_EXPERTS = 16
EXPERT_HIDDEN = 1024
CAPACITY_FACTOR = 2
N_BRANCHES = 3
N_MOD = 6
ROPE_BASE = 10000.0
EPS = 1e-6
NEG_INF = -1e30
RET_QK = RET_HEADS * RET_DK
RET_V = RET_HEADS * RET_DV
WIN_Q = WIN_HEADS * WIN_HEAD_DIM
WIN_KV = WIN_KV_HEADS * WIN_HEAD_DIM
NA_W = NA_HEADS * NA_HEAD_DIM
IN_SPLITS = (('q_r', RET_QK), ('k_r', RET_QK), ('v_r', RET_V), ('g_r', RET_V),
             ('q_w', WIN_Q), ('k_w', WIN_KV), ('v_w', WIN_KV),
             ('q_n', NA_W), ('k_n', NA_W), ('v_n', NA_W),
             ('gates', N_BRANCHES * D_MODEL))
IN_COLS = 2 * RET_QK + 2 * RET_V + WIN_Q + 2 * WIN_KV + 3 * NA_W + N_BRANCHES * D_MODEL
KV_NAMES = ('k_r', 'v_r', 'k_w', 'v_w', 'k_n', 'v_n')

kernel_name = 'hybrid_retention_window_natten_ecmoe'


def rms_norm(x, g):
    xf = x.astype(jnp.float32)
    y = xf * lax.rsqrt(jnp.mean(xf * xf, axis=-1, keepdims=True) + EPS)
    return (y * g.astype(jnp.float32)).astype(x.dtype)


def modulate(h, shift, scale):
    return h * (1 + scale[:, None, :]) + shift[:, None, :]


def column_layout():
    layout, off = {}, 0
    for name, size in IN_SPLITS:
        layout[name] = (off, size)
        off += size
    return layout


def split_columns(p):
    return {n: p[..., o:o + s] for n, (o, s) in column_layout().items()}


def heads(t, n_heads):
    return t.reshape(t.shape[0], t.shape[1], n_heads, -1)


def axial_rope(x):
    n, d = x.shape[1], x.shape[-1]
    nf = d // 4
    t = jnp.arange(n)
    row = (t // GRID_W).astype(jnp.float32)
    col = (t % GRID_W).astype(jnp.float32)
    inv = ROPE_BASE ** (-jnp.arange(nf, dtype=jnp.float32) / nf)
    ang = jnp.concatenate([row[:, None] * inv, col[:, None] * inv], axis=-1)
    cos = jnp.cos(ang)[None, :, None, :]
    sin = jnp.sin(ang)[None, :, None, :]
    xf = x.astype(jnp.float32)
    x1, x2 = xf[..., :d // 2], xf[..., d // 2:]
    return jnp.concatenate([x1 * cos - x2 * sin, x1 * sin + x2 * cos], axis=-1).astype(x.dtype)


def retention_final_state(k, v, log_gamma):
    n = k.shape[2]
    w = jnp.exp((n - 1 - jnp.arange(n, dtype=jnp.float32))[None, :] * log_gamma[:, None])
    return jnp.einsum('bhnd,bhne->bhde', k * w[None, :, :, None], v)


def retention_chunkwise(q, k, v, log_gamma, state0, include_diag):
    b, h, n, dk = q.shape
    dv = v.shape[-1]
    ch = RET_CHUNK
    nc = n // ch
    qc = q.reshape(b, h, nc, ch, dk)
    kc = k.reshape(b, h, nc, ch, dk)
    vc = v.reshape(b, h, nc, ch, dv)
    i = jnp.arange(ch, dtype=jnp.float32)
    diff = i[:, None] - i[None, :]
    lg = log_gamma[:, None, None]
    dmat = jnp.where(diff >= (0.0 if include_diag else 1.0), jnp.exp(jnp.maximum(diff, 0.0) * lg), 0.0)
    inner = jnp.einsum('bhnid,bhnjd->bhnij', qc, kc) * dmat[None, :, None]
    inner = jnp.einsum('bhnij,bhnje->bhnie', inner, vc)
    zeta = jnp.exp((ch - 1 - i)[None, :] * log_gamma[:, None])
    s = jnp.einsum('bhnjd,bhnje->nbhde', kc * zeta[None, :, None, :, None], vc)
    chunk_decay = jnp.exp(ch * log_gamma)[None, :, None, None]

    def step(r, s_n):
        return chunk_decay * r + s_n, r

    _, r_prev = lax.scan(step, state0, s)
    xi = jnp.exp((i + 1)[None, :] * log_gamma[:, None])
    cross = jnp.einsum('bhnid,nbhde->bhnie', qc, r_prev) * xi[None, :, None, :, None]
    return (inner + cross).reshape(b, h, n, dv)


def bidirectional_retention(q, k, v, lg_f, lg_b, state_f, state_b):
    flip = lambda t: jnp.flip(t, axis=2)
    fwd = retention_chunkwise(q, k, v, lg_f, state_f, True)
    bwd = retention_chunkwise(flip(q), flip(k), flip(v), lg_b, state_b, False)
    return fwd + flip(bwd)


def retention_readout(y, g, gn_gain):
    mu = jnp.mean(y, axis=-1, keepdims=True)
    var = jnp.mean(jnp.square(y - mu), axis=-1, keepdims=True)
    y = (y - mu) * lax.rsqrt(var + EPS)
    y = jnp.swapaxes(y, 1, 2).reshape(y.shape[0], y.shape[2], -1) * gn_gain.astype(jnp.float32)
    return (y * jax.nn.silu(g.astype(jnp.float32))).astype(g.dtype)


def retention_mixer(px, pc, decay_logit, gn_gain, need_ctx):
    lg_f = jax.nn.log_sigmoid(decay_logit[0].astype(jnp.float32))
    lg_b = jax.nn.log_sigmoid(decay_logit[1].astype(jnp.float32))
    to_bhnd = lambda t: jnp.swapaxes(t, 1, 2).astype(jnp.float32)
    k_scale = RET_DK ** -0.5
    qx = to_bhnd(axial_rope(heads(px['q_r'], RET_HEADS)))
    kx = to_bhnd(axial_rope(heads(px['k_r'], RET_HEADS))) * k_scale
    vx = to_bhnd(heads(px['v_r'], RET_HEADS))
    kc = to_bhnd(heads(pc['k_r'], RET_HEADS)) * k_scale
    vc = to_bhnd(heads(pc['v_r'], RET_HEADS))
    flip = lambda t: jnp.flip(t, axis=2)
    state_f = retention_final_state(kc, vc, lg_f)
    state_b = retention_final_state(flip(kc), flip(vc), lg_b)
    yx = retention_readout(bidirectional_retention(qx, kx, vx, lg_f, lg_b, state_f, state_b), px['g_r'], gn_gain)
    yc = None
    if need_ctx:
        qc = to_bhnd(heads(pc['q_r'], RET_HEADS))
        zero = jnp.zeros(state_f.shape, jnp.float32)
        yc = retention_readout(bidirectional_retention(qc, kc, vc, lg_f, lg_b, zero, zero), pc['g_r'], gn_gain)
    return yx, yc


def context_self_attention(q, k, v, sink):
    b, l, hq, d = q.shape
    hkv = k.shape[2]
    g = hq // hkv
    qg = q.reshape(b, l, hkv, g, d)
    s = jnp.einsum('bqhgd,bkhd->bhgqk', qg, k).astype(jnp.float32) * d ** -0.5
    if sink is not None:
        s_sink = jnp.broadcast_to(sink.astype(jnp.float32).reshape(1, hkv, g, 1, 1), s.shape[:-1] + (1,))
        s = jnp.concatenate([s, s_sink], axis=-1)
    p = jax.nn.softmax(s, axis=-1)[..., :l].astype(v.dtype)
    return jnp.einsum('bhgqk,bkhd->bqhgd', p, v).reshape(b, l, hq * d)


def banded_window_attention(q, k, v, kc, vc, sink):
    b, n, hq, d = q.shape
    hkv = k.shape[2]
    g = hq // hkv
    l = kc.shape[1]
    nb = n // WIN_BLOCK
    pad = ((0, 0), (WIN_BLOCK, WIN_BLOCK), (0, 0), (0, 0))

    def band(t):
        tb = jnp.pad(t, pad).reshape(b, nb + 2, WIN_BLOCK, hkv, d)
        return jnp.concatenate([tb[:, :-2], tb[:, 1:-1], tb[:, 2:]], axis=2)

    kb, vb = band(k), band(v)
    qb = q.reshape(b, nb, WIN_BLOCK, hkv, g, d)
    scale = d ** -0.5
    s_loc = jnp.einsum('bnqhgd,bnkhd->bnhgqk', qb, kb).astype(jnp.float32) * scale
    s_ctx = jnp.einsum('bnqhgd,blhd->bnhgql', qb, kc).astype(jnp.float32) * scale
    qpos = jnp.arange(WIN_BLOCK)[:, None]
    kpos = jnp.arange(3 * WIN_BLOCK)[None, :] - WIN_BLOCK
    kabs = jnp.arange(nb)[:, None, None] * WIN_BLOCK + kpos[None]
    valid = (jnp.abs(kpos - qpos) <= WINDOW)[None] & (kabs >= 0) & (kabs < n)
    s_loc = jnp.where(valid[None, :, None, None], s_loc, NEG_INF)
    s_sink = jnp.broadcast_to(sink.astype(jnp.float32).reshape(1, 1, hkv, g, 1, 1), s_ctx.shape[:-1] + (1,))
    p = jax.nn.softmax(jnp.concatenate([s_loc, s_ctx, s_sink], axis=-1), axis=-1)
    kl = 3 * WIN_BLOCK
    p_loc = p[..., :kl].astype(v.dtype)
    p_ctx = p[..., kl:kl + l].astype(v.dtype)
    out = jnp.einsum('bnhgqk,bnkhd->bnqhgd', p_loc, vb) + jnp.einsum('bnhgql,blhd->bnqhgd', p_ctx, vc)
    return out.reshape(b, n, hq * d)


def window_mixer(px, pc, sink, need_ctx):
    q = axial_rope(heads(px['q_w'], WIN_HEADS))
    k = axial_rope(heads(px['k_w'], WIN_KV_HEADS))
    v = heads(px['v_w'], WIN_KV_HEADS)
    kc = heads(pc['k_w'], WIN_KV_HEADS)
    vc = heads(pc['v_w'], WIN_KV_HEADS)
    yx = banded_window_attention(q, k, v, kc, vc, sink)
    yc = context_self_attention(heads(pc['q_w'], WIN_HEADS), kc, vc, sink) if need_ctx else None
    return yx, yc


def neighbourhood_attention(q, k, v, kc, vc, rpb):
    b, n, h, d = q.shape
    rows = n // GRID_W
    kr = min(NA_ROWS_MAX, rows)
    r = jnp.arange(rows)
    row_idx = jnp.clip(r - kr // 2, 0, rows - kr)[:, None] + jnp.arange(kr)[None, :]
    cq = jnp.arange(GRID_W)
    col_start = jnp.clip(cq - NA_COLS // 2, 0, GRID_W - NA_COLS)
    col_valid = (cq[None, :] >= col_start[:, None]) & (cq[None, :] < col_start[:, None] + NA_COLS)
    ri = row_idx - r[:, None] + NA_ROWS_MAX - 1
    ci = jnp.clip(cq[None, :] - cq[:, None], -(NA_COLS - 1), NA_COLS - 1) + NA_COLS - 1
    bias = rpb.astype(jnp.float32)[:, ri[:, None, :, None], ci[None, :, None, :]]
    qg = q.reshape(b, rows, GRID_W, h, d)
    k_rows = k.reshape(b, rows, GRID_W, h, d)[:, row_idx]
    v_rows = v.reshape(b, rows, GRID_W, h, d)[:, row_idx]
    scale = d ** -0.5
    s_loc = jnp.einsum('brchd,brkwhd->bhrckw', qg, k_rows).astype(jnp.float32) * scale + bias[None]
    s_loc = jnp.where(col_valid[:, None, :], s_loc, NEG_INF).reshape(b, h, rows, GRID_W, kr * GRID_W)
    s_ctx = jnp.einsum('brchd,blhd->bhrcl', qg, kc).astype(jnp.float32) * scale
    p = jax.nn.softmax(jnp.concatenate([s_loc, s_ctx], axis=-1), axis=-1)
    nl = kr * GRID_W
    p_loc = p[..., :nl].reshape(b, h, rows, GRID_W, kr, GRID_W).astype(v.dtype)
    p_ctx = p[..., nl:].astype(v.dtype)
    out = jnp.einsum('bhrckw,brkwhd->brchd', p_loc, v_rows) + jnp.einsum('bhrcl,blhd->brchd', p_ctx, vc)
    return out.reshape(b, n, h * d)


def na_mixer(px, pc, rpb, need_ctx):
    q = heads(px['q_n'], NA_HEADS)
    k = heads(px['k_n'], NA_HEADS)
    v = heads(px['v_n'], NA_HEADS)
    kc = heads(pc['k_n'], NA_HEADS)
    vc = heads(pc['v_n'], NA_HEADS)
    yx = neighbourhood_attention(q, k, v, kc, vc, rpb)
    yc = context_self_attention(heads(pc['q_n'], NA_HEADS), kc, vc, None) if need_ctx else None
    return yx, yc


def gated_merge(gates, ya, yb, yc, w_ret, w_win, w_na, w_out):
    ga, gb, gc = jnp.split(jax.nn.sigmoid(gates), N_BRANCHES, axis=-1)
    return (ga * (ya @ w_ret) + gb * (yb @ w_win) + gc * (yc @ w_na)) @ w_out


def hybrid_mixer(hx, hc, w_in, decay_logit, gn_gain, w_ret, sink, w_win, rpb, w_na, w_out, need_ctx):
    px = split_columns(hx @ w_in)
    if need_ctx:
        pc = split_columns(hc @ w_in)
    else:
        lay = column_layout()
        pc = {nm: hc @ w_in[:, lay[nm][0]:lay[nm][0] + lay[nm][1]] for nm in KV_NAMES}
    ya, yca = retention_mixer(px, pc, decay_logit, gn_gain, need_ctx)
    yb, ycb = window_mixer(px, pc, sink, need_ctx)
    yc, ycc = na_mixer(px, pc, rpb, need_ctx)
    mx = gated_merge(px['gates'], ya, yb, yc, w_ret, w_win, w_na, w_out)
    mc = gated_merge(pc['gates'], yca, ycb, ycc, w_ret, w_win, w_na, w_out) if need_ctx else None
    return mx, mc


def expert_choice_ffn(h, w_router, w_gate, w_up, w_down):
    b, n, d = h.shape
    cap = CAPACITY_FACTOR * n // N_EXPERTS
    aff = jax.nn.softmax((h @ w_router).astype(jnp.float32), axis=-1)
    gate, idx = lax.top_k(jnp.swapaxes(aff, 1, 2), cap)
    bidx = jnp.arange(b)[:, None, None]
    xe = h[bidx, idx]
    hid = jax.nn.silu(jnp.einsum('becd,edf->becf', xe, w_gate)) * jnp.einsum('becd,edf->becf', xe, w_up)
    ye = jnp.einsum('becf,efd->becd', hid, w_down) * gate[..., None].astype(h.dtype)
    return jnp.zeros_like(h).at[bidx, idx].add(ye)


def setup_inputs(seed: int = 0) -> dict:
    key = jax.random.key(seed)
    ks = jax.random.split(key, 24)
    f32 = jnp.float32
    d = D_MODEL
    nrm = lambda k, shape, s: jax.random.normal(k, shape, f32) * s
    gamma0 = 1.0 - 2.0 ** (-5.0 - np.arange(RET_HEADS))
    logit0 = jnp.asarray(np.log(gamma0 / (1.0 - gamma0)).astype(np.float32))
    return {
        'x': nrm(ks[0], (BATCH, SEQ, d), 1.0),
        'c': nrm(ks[1], (BATCH, d), 1.0),
        'ctx': nrm(ks[2], (BATCH, CTX_LEN, d), 1.0),
        'c_ctx': nrm(ks[3], (d,), 1.0),
        'w_mod': nrm(ks[4], (DEPTH, d, N_MOD * d), 0.5 * d ** -0.5),
        'b_mod': nrm(ks[5], (DEPTH, N_MOD * d), 0.02),
        'g_mix': 1.0 + nrm(ks[6], (DEPTH, d), 0.02),
        'g_ffn': 1.0 + nrm(ks[7], (DEPTH, d), 0.02),
        'w_in': nrm(ks[8], (DEPTH, d, IN_COLS), d ** -0.5),
        'ret_decay_logit': logit0[None, None, :] + nrm(ks[9], (DEPTH, 2, RET_HEADS), 0.1),
        'ret_gn': 1.0 + nrm(ks[10], (DEPTH, RET_V), 0.02),
        'w_ret': nrm(ks[11], (DEPTH, RET_V, d), RET_V ** -0.5),
        'win_sink': nrm(ks[12], (DEPTH, WIN_HEADS), 1.0),
        'w_win': nrm(ks[13], (DEPTH, WIN_Q, d), WIN_Q ** -0.5),
        'na_rpb': nrm(ks[14], (DEPTH, NA_HEADS, 2 * NA_ROWS_MAX - 1, 2 * NA_COLS - 1), 0.1),
        'w_na': nrm(ks[15], (DEPTH, NA_W, d), NA_W ** -0.5),
        'w_out': nrm(ks[16], (DEPTH, d, d), d ** -0.5),
        'w_router': nrm(ks[17], (DEPTH, d, N_EXPERTS), d ** -0.5),
        'w_exp_gate': nrm(ks[18], (DEPTH, N_EXPERTS, d, EXPERT_HIDDEN), d ** -0.5),
        'w_exp_up': nrm(ks[19], (DEPTH, N_EXPERTS, d, EXPERT_HIDDEN), d ** -0.5),
        'w_exp_down': nrm(ks[20], (DEPTH, N_EXPERTS, EXPERT_HIDDEN, d), EXPERT_HIDDEN ** -0.5),
        'g_final': 1.0 + nrm(ks[21], (d,), 0.02),
    }


def reference(x, c, ctx, c_ctx, w_mod, b_mod, g_mix, g_ffn, w_in, ret_decay_logit, ret_gn, w_ret,
              win_sink, w_win, na_rpb, w_na, w_out, w_router, w_exp_gate, w_exp_up, w_exp_down, g_final):
    d = x.shape[-1]
    sc = jax.nn.silu(c)
    scc = jax.nn.silu(c_ctx)[None]
    for layer in range(DEPTH):
        need_ctx = layer < DEPTH - 1
        mod_x = (sc @ w_mod[layer] + b_mod[layer]).reshape(-1, N_MOD, d)
        n_mod_c = N_MOD if need_ctx else 2
        mod_c = (scc @ w_mod[layer][:, :n_mod_c * d] + b_mod[layer][:n_mod_c * d]).reshape(1, n_mod_c, d)
        hx = modulate(rms_norm(x, g_mix[layer]), mod_x[:, 0], mod_x[:, 1])
        hc = modulate(rms_norm(ctx, g_mix[layer]), mod_c[:, 0], mod_c[:, 1])
        mx, mc = hybrid_mixer(hx, hc, w_in[layer], ret_decay_logit[layer], ret_gn[layer], w_ret[layer],
                              win_sink[layer], w_win[layer], na_rpb[layer], w_na[layer], w_out[layer], need_ctx)
        x = x + mod_x[:, 2, None] * mx
        hx = modulate(rms_norm(x, g_ffn[layer]), mod_x[:, 3], mod_x[:, 4])
        x = x + mod_x[:, 5, None] * expert_choice_ffn(hx, w_router[layer], w_exp_gate[layer],
                                                      w_exp_up[layer], w_exp_down[layer])
        if need_ctx:
            ctx = ctx + mod_c[:, 2, None] * mc
            hc = modulate(rms_norm(ctx, g_ffn[layer]), mod_c[:, 3], mod_c[:, 4])
            ctx = ctx + mod_c[:, 5, None] * expert_choice_ffn(hc, w_router[layer], w_exp_gate[layer],
                                                              w_exp_up[layer], w_exp_down[layer])
    return rms_norm(x, g_final)
```

```python
import numpy as np
from contextlib import ExitStack
import concourse.bass as bass
import concourse.mybir as mybir
from concourse.bass_utils import run_bass_kernel_spmd

F32 = mybir.dt.float32
BF16 = mybir.dt.bfloat16
AF = mybir.ActivationFunctionType
ALU = mybir.AluOpType
AX = mybir.AxisListType

ENGS = ('pe', 'act', 'dve', 'pool', 'sp')
SAME_ENGINE_SYNC = True
N_DMA_SEMS = 40
NEG = -30000.0

CFG = dict(D=1024, NCH=16, NR=8, NE=16, EH=1024)


class T:
    def __init__(self, name, t, nslots=1):
        self.name, self.t, self.nslots = name, t, nslots

    def k(self, *idx):
        return [(self.name, i % self.nslots) for i in idx]

    def all(self):
        return [(self.name, i) for i in range(self.nslots)]

    def __getitem__(self, item):
        return self.t[item]


class Sched:
    def __init__(self, nc, es):
        self.nc, self.es = nc, es
        self.q = {e: [] for e in ENGS}
        self.cnt = {e: 0 for e in ENGS}
        self.sem = {e: es.enter_context(nc.semaphore('s_' + e)) for e in ENGS if e != 'sp'}
        self.dsem = [es.enter_context(nc.semaphore('d%d' % i)) for i in range(N_DMA_SEMS)]
        self.dval = [0] * N_DMA_SEMS
        self.dnext = 0
        self.seen = {e: {} for e in ENGS}
        self.w, self.r = {}, {}
        self.final = {}
        self.uid = 0
        self.freed = {}

    def _reg(self, t):
        fl = [(s_, v, e) for s_, (v, e) in self.freed.items()]
        for key in t.all():
            self.r[key] = list(fl)
        return t

    def scope(self):
        return _Scope(self)

    def sb(self, name, shape, dt, nslots=1):
        self.uid += 1
        name = '%s_%d' % (name, self.uid)
        return self._reg(T(name, self.es.enter_context(self.nc.sbuf_tensor(name, list(shape), dt)), nslots))

    def sbs(self, es, name, shape, dt, nslots=1):
        self.uid += 1
        name = '%s_%d' % (name, self.uid)
        t = self._reg(T(name, es.enter_context(self.nc.sbuf_tensor(name, list(shape), dt)), nslots))
        es._tl.append(t)
        return t

    def ps(self, name, shape, dt=F32, nslots=1):
        return T(name, self.es.enter_context(self.nc.psum_tensor(name, list(shape), dt)), nslots)

    def op(self, eng, fn, r=(), w=(), dma=False):
        r, w = list(r), list(w)
        deps = []
        for b in r:
            if b in self.w:
                deps.append(self.w[b])
        for b in w:
            if b in self.w:
                deps.append(self.w[b])
            deps.extend(self.r.get(b, ()))
        seen = self.seen[eng]
        waits = {}
        for (semid, val, src) in deps:
            if src == eng and not isinstance(semid, int):
                if eng == 'pe' or not SAME_ENGINE_SYNC:
                    continue
            if seen.get(semid, 0) < val:
                seen[semid] = val
                waits[semid] = val
        if dma:
            half = N_DMA_SEMS // 2
            base = 0 if eng == 'pool' else half
            self.dnx = getattr(self, 'dnx', {})
            si = base + self.dnx.get(eng, 0)
            self.dnx[eng] = (self.dnx.get(eng, 0) + 1) % half
            prev = self.dval[si]
            if prev > 0 and seen.get(si, 0) < prev:
                seen[si] = prev
                waits[si] = prev
            self.dval[si] = prev + 16
            tok = (si, prev + 16, eng)
            inc = (si, 16)
        else:
            self.cnt[eng] += 1
            tok = (eng, self.cnt[eng], eng)
            inc = (eng, 1)
        self.q[eng].append((list(waits.items()), fn, inc))
        for b in w:
            self.w[b] = tok
            self.r[b] = []
        for b in r:
            self.r.setdefault(b, []).append(tok)

    def _sem(self, s):
        return self.dsem[s] if isinstance(s, int) else self.sem[s]

    def finish(self, eng, keys):
        d = self.final.setdefault(eng, {})
        for b in keys:
            if b in self.w:
                s, v, _ = self.w[b]
                d[s] = max(d.get(s, 0), v)

    def emit(self):
        hn = {'pe': 'tensor', 'act': 'scalar', 'dve': 'vector', 'pool': 'gpsimd', 'sp': 'sync'}
        with self.nc.Block() as block:
            for eng in ENGS:
                q, fw = self.q[eng], self.final.get(eng, {})
                if not q and not fw:
                    continue

                def body(h, q=q, fw=fw):
                    for waits, fn, inc in q:
                        for s, v in waits:
                            h.wait_ge(self._sem(s), v)
                        fn(h).then_inc(self._sem(inc[0]), inc[1])
                    for s, v in fw.items():
                        h.wait_ge(self._sem(s), v)
                getattr(block, hn[eng])(body)


class _Scope(ExitStack):
    def __init__(self, S):
        super().__init__()
        self.S = S
        self._tl = []

    def __exit__(self, *a):
        S = self.S
        for t in self._tl:
            for key in t.all():
                toks = list(S.r.get(key, []))
                if key in S.w:
                    toks.append(S.w[key])
                for (s_, v, e) in toks:
                    if S.freed.get(s_, (0, e))[0] < v:
                        S.freed[s_] = (v, e)
        return super().__exit__(*a)


def col_layout(D):
    names = [('q_r', 512), ('k_r', 512), ('v_r', 512), ('g_r', 512), ('q_w', 512), ('k_w', 128), ('v_w', 128),
             ('q_n', 512), ('k_n', 512), ('v_n', 512), ('gates', 3 * D)]
    lay, off = {}, 0
    for n, s in names:
        lay[n] = off
        off += s
    return lay, off


class Ctx:
    pass


def dt_in(nc, name, shape, dt=F32):
    return nc.dram_tensor(name, list(shape), dt, kind="ExternalInput").ap()


def dt_out(nc, name, shape, dt=F32):
    return nc.dram_tensor(name, list(shape), dt, kind="ExternalOutput").ap()


def common_setup(S, g):
    nc = g.nc
    g.ident = S.sb('ident', [128, 128], BF16)
    g.identf = S.sb('identf', [128, 128], F32)
    g.ones = S.sb('ones', [128, 128], F32)
    for t in (g.ident, g.identf):
        S.op('pool', lambda e, t=t: e.memset(t[:], 0.0), w=t.all())
        S.op('pool', lambda e, t=t: e.affine_select(out=t[:], in_=t[:], pattern=[[-1, 128]], compare_op=ALU.not_equal,
                                                    fill=1.0, base=0, channel_multiplier=1), r=t.all(), w=t.all())
    S.op('pool', lambda e: e.memset(g.ones[:], 1.0), w=g.ones.all())
    g.psb = [S.ps('ps%d' % i, [128, 512], F32) for i in range(7)]
    g.pst = S.ps('pst', [128, 1024], BF16)


def load_bc(S, g, dst, src_row, eng='sp'):
    S.op(eng, lambda e: e.dma_start(out=dst[:], in_=src_row.partition_broadcast(128)), w=dst.all(), dma=True)


def load_w(S, g, dst, src, kdim):
    S.op('pool', lambda e: e.dma_start(out=dst[:], in_=src.rearrange("(c p) n -> p c n", p=128)), w=dst.all(), dma=True)


def mod_bc(S, g, es, m, s, out):
    D, KD = g.D, g.KD
    with S.scope() as es2:
        wm = S.sbs(es2, 'wm', [128, KD, D], BF16)
        bb = S.sbs(es2, 'bb', [128, D], F32)
        load_w(S, g, wm, g.w_mod[:, m * D:(m + 1) * D], KD)
        load_bc(S, g, bb, g.b_mod[0:1, m * D:(m + 1) * D])
        BW = min(512, D)
        for hb in range(D // BW):
            ps = g.psb[hb % 2]
            for kc in range(KD):
                S.op('pe', lambda e, kc=kc, hb=hb, ps=ps: e.matmul(ps[:, 0:BW], lhsT=g.crep[:, s, kc, :], rhs=wm[:, kc, hb * BW:(hb + 1) * BW],
                                                                   start=(kc == 0), stop=(kc == KD - 1)),
                     r=g.crep.all() + wm.all(), w=ps.all())
            S.op('dve', lambda e, hb=hb, ps=ps: e.tensor_tensor(out=out[:, hb * BW:(hb + 1) * BW], in0=ps[:, 0:BW], in1=bb[:, hb * BW:(hb + 1) * BW], op=ALU.add),
                 r=ps.all() + bb.all(), w=out.all())


def setup_c(S, g):
    KD = g.KD
    craw = S.sb('craw', [128, 2, KD], F32)
    csil = S.sb('csil', [128, 2, KD], F32)
    g.crep = S.sb('crep', [128, 2, KD, 128], BF16)
    S.op('sp', lambda e: e.dma_start(out=craw[:], in_=g.c2.rearrange("s (k p) -> p s k", p=128), allow_slow_non_contiguous=True), w=craw.all(), dma=True)
    S.op('act', lambda e: e.activation(out=csil[:], in_=craw[:], func=AF.Silu), r=craw.all(), w=csil.all())
    for s in range(2):
        for k in range(KD):
            S.op('dve', lambda e, s=s, k=k: e.tensor_scalar(out=g.crep[:, s, k, :], in0=g.ones[:], scalar1=csil[:, s, k:k + 1], scalar2=None, op0=ALU.mult),
                 r=csil.all() + g.ones.all(), w=g.crep.all())


def norm_chunk(S, g, xrows, A, B, hT, ci, wk, rdeps=()):
    D, KD = g.D, g.KD
    xt, junk, xb, st = wk
    S.op('sp', lambda e: e.dma_start(out=xt[:], in_=xrows), r=list(rdeps), w=xt.all(), dma=True)
    S.op('act', lambda e: e.activation(out=junk[:], in_=xt[:], func=AF.Square, accum_out=st[:, 0:1]), r=xt.all(), w=junk.all() + st.k(0))
    S.op('dve', lambda e: e.tensor_scalar(out=st[:, 1:2], in0=st[:, 0:1], scalar1=1.0 / D, scalar2=1e-6, op0=ALU.mult, op1=ALU.add), r=st.k(0), w=st.k(0))
    S.op('act', lambda e: e.activation(out=st[:, 2:3], in_=st[:, 1:2], func=AF.Sqrt), r=st.k(0), w=st.k(0))
    S.op('dve', lambda e: e.reciprocal(out=st[:, 3:4], in_=st[:, 2:3]), r=st.k(0), w=st.k(0))
    S.op('dve', lambda e: e.scalar_tensor_tensor(out=junk[:], in0=xt[:], scalar=st[:, 3:4], in1=A[:], op0=ALU.mult, op1=ALU.mult),
         r=xt.all() + st.k(0) + A.all(), w=junk.all())
    S.op('dve', lambda e: e.tensor_tensor(out=xb[:], in0=junk[:], in1=B[:], op=ALU.add), r=junk.all() + B.all(), w=xb.all())
    for kd in range(KD):
        S.op('pe', lambda e, kd=kd: e.transpose(out=g.pst[:, kd * 128:(kd + 1) * 128], in_=xb[:, kd * 128:(kd + 1) * 128], identity=g.ident[:]),
             r=xb.all() + g.ident.all(), w=g.pst.all())
    S.op('act', lambda e: e.activation(out=hT[:, :, ci * 128:(ci + 1) * 128], in_=g.pst[:, 0:KD * 128].rearrange("p (k t) -> p k t", k=KD), func=AF.Identity),
         r=g.pst.all(), w=hT.k(ci))


def norm_all(S, g, es, xsrc, gvec, m_shift, m_scale, hT, chunks, deps=None):
    D = g.D
    with S.scope() as es2:
        gb = S.sbs(es2, 'gb', [128, D], F32)
        load_bc(S, g, gb, gvec)
        AB = {}
        for s in range(2):
            A = S.sbs(es2, 'A%d' % s, [128, D], F32)
            B = S.sbs(es2, 'B%d' % s, [128, D], F32)
            mod_bc(S, g, es2, m_scale, s, A)
            mod_bc(S, g, es2, m_shift, s, B)
            S.op('dve', lambda e, A=A: e.scalar_tensor_tensor(out=A[:], in0=A[:], scalar=1.0, in1=gb[:], op0=ALU.add, op1=ALU.mult), r=A.all() + gb.all(), w=A.all())
            AB[s] = (A, B)
        wk = (S.sbs(es2, 'xt', [128, D], F32), S.sbs(es2, 'junk', [128, D], F32), S.sbs(es2, 'xb', [128, D], BF16), S.sbs(es2, 'st', [128, 4], F32))
        for ci in chunks:
            s = 0 if ci < g.NCH else 1
            norm_chunk(S, g, xsrc[ci * 128:(ci + 1) * 128, :], AB[s][0], AB[s][1], hT, ci, wk, deps(ci) if deps else [])


def proj_fm(S, g, ps, W, ntile, hT, tok0, ntok, col0=0, M=128):
    KD = g.KD
    for j in range(ntile):
        for kc in range(KD):
            S.op('pe', lambda e, j=j, kc=kc: e.matmul(ps[0:M, j * ntok:(j + 1) * ntok], lhsT=W[:, kc, col0 + j * M:col0 + (j + 1) * M],
                                                      rhs=hT[:, kc, tok0:tok0 + ntok], start=(kc == 0), stop=(kc == KD - 1)),
                 r=W.all() + hT.k(tok0 // 128), w=ps.all())


def proj_tm(S, g, ps, W, ncols, hT, ci, col0=0):
    KD = g.KD
    for kc in range(KD):
        S.op('pe', lambda e, kc=kc: e.matmul(ps[:, 0:ncols], lhsT=hT[:, kc, ci * 128:(ci + 1) * 128], rhs=W[:, kc, col0:col0 + ncols],
                                             start=(kc == 0), stop=(kc == KD - 1)),
             r=W.all() + hT.k(ci), w=ps.all())


def out_chain(S, g, y, Wb, Wg, hT, ci, wk):
    D, KD = g.D, g.KD
    yT, gs, gm, xt, tmp = wk
    s = 0 if ci < g.NCH else 1
    for c in range(4):
        S.op('pe', lambda e, c=c: e.transpose(out=g.pst[:, c * 128:(c + 1) * 128], in_=y[:, c * 128:(c + 1) * 128], identity=g.ident[:]),
             r=y.all() + g.ident.all(), w=g.pst.all())
    S.op('act', lambda e: e.activation(out=yT[:], in_=g.pst[:, 0:512].rearrange("p (c t) -> p c t", c=4), func=AF.Identity), r=g.pst.all(), w=yT.all())
    nb = (KD * 128 + 511) // 512
    pp = [g.psb[0], g.psb[1]]
    pg = [g.psb[2], g.psb[3]]
    for kd in range(KD):
        b, o = divmod(kd, 4)
        for c in range(4):
            S.op('pe', lambda e, kd=kd, c=c, b=b, o=o: e.matmul(pp[b][:, o * 128:(o + 1) * 128], lhsT=Wb[:, c, kd * 128:(kd + 1) * 128], rhs=yT[:, c, :],
                                                              start=(c == 0), stop=(c == 3)), r=Wb.all() + yT.all(), w=pp[b].all())
        for kc in range(KD):
            S.op('pe', lambda e, kd=kd, kc=kc, b=b, o=o: e.matmul(pg[b][:, o * 128:(o + 1) * 128], lhsT=Wg[:, kc, kd * 128:(kd + 1) * 128],
                                                                rhs=hT[:, kc, ci * 128:(ci + 1) * 128], start=(kc == 0), stop=(kc == KD - 1)),
                 r=Wg.all() + hT.k(ci), w=pg[b].all())
    for b in range(nb):
        n = min(4, KD - 4 * b) * 128
        S.op('act', lambda e, b=b, n=n: e.activation(out=gs[:, b * 512:b * 512 + n], in_=pg[b][:, 0:n], func=AF.Sigmoid), r=pg[b].all(), w=gs.all())
        S.op('dve', lambda e, b=b, n=n: e.tensor_tensor(out=gm[:, b * 512:b * 512 + n], in0=pp[b][:, 0:n], in1=gs[:, b * 512:b * 512 + n], op=ALU.mult),
             r=pp[b].all() + gs.all(), w=gm.all())
    S.op('sp', lambda e: e.dma_start(out=xt[:], in_=g.x1[ci * 128:(ci + 1) * 128, :]), r=[('x1', ci)], w=xt.all(), dma=True)
    BW = min(512, D)
    for hb in range(D // BW):
        po = g.psb[4 + hb % 2]
        for kd in range(KD):
            S.op('pe', lambda e, kd=kd, hb=hb, po=po: e.matmul(po[:, 0:BW], lhsT=gm[:, kd * 128:(kd + 1) * 128], rhs=g.Wout[:, kd, hb * BW:(hb + 1) * BW],
                                                             start=(kd == 0), stop=(kd == KD - 1)), r=gm.all() + g.Wout.all(), w=po.all())
        S.op('dve', lambda e, hb=hb, po=po: e.tensor_tensor(out=tmp[:, hb * BW:(hb + 1) * BW], in0=po[:, 0:BW], in1=g.mod2[s][:, hb * BW:(hb + 1) * BW], op=ALU.mult),
             r=po.all() + g.mod2[s].all(), w=tmp.all())
    S.op('pool', lambda e: e.tensor_tensor(out=xt[:], in0=xt[:], in1=tmp[:], op=ALU.add), r=xt.all() + tmp.all(), w=xt.all())
    S.op('sp', lambda e: e.dma_start(out=g.x1[ci * 128:(ci + 1) * 128, :], in_=xt[:]), r=xt.all(), w=[('x1', ci)], dma=True)


def chain_wk(S, es, g):
    return (S.sbs(es, 'yT', [128, 4, 128], BF16), S.sbs(es, 'gs', [128, max(g.KD * 128, 512)], F32), S.sbs(es, 'gm', [128, g.KD * 128], BF16),
            S.sbs(es, 'xt', [128, g.D], F32), S.sbs(es, 'tmp', [128, g.D], F32))


def retention(S, g, hT, need_ctx):
    D, KD, NCH, NR = g.D, g.KD, g.NCH, g.NR
    TCH = NCH + 2
    lay = g.lay
    with S.scope() as es:
        sb = lambda n, sh, dt, ns=1: S.sbs(es, n, sh, dt, ns)
        rc = sb('rc', [128, 2 + 128 * 6], F32)
        S.op('sp', lambda e: e.dma_start(out=rc[:], in_=g.rconst), w=rc.all(), dma=True)
        pcol = lambda i: rc[:, i:i + 1]
        tab = lambda i: rc[:, 2 + i * 128:2 + (i + 1) * 128]
        lg = sb('lg', [128, 8], F32)
        load_bc(S, g, lg, g.decay[0:1, :])
        S.op('act', lambda e: e.activation(out=lg[:], in_=lg[:], func=AF.Exp, scale=-1.0), r=lg.all(), w=lg.all())
        S.op('dve', lambda e: e.tensor_scalar(out=lg[:], in0=lg[:], scalar1=1.0, scalar2=None, op0=ALU.add), r=lg.all(), w=lg.all())
        S.op('act', lambda e: e.activation(out=lg[:], in_=lg[:], func=AF.Ln), r=lg.all(), w=lg.all())
        S.op('dve', lambda e: e.tensor_scalar(out=lg[:], in0=lg[:], scalar1=-1.0, scalar2=None, op0=ALU.mult), r=lg.all(), w=lg.all())
        Z = sb('Z', [128, 8], F32)
        G128 = sb('G128', [128, 8], F32)
        for d_ in range(2):
            for h in range(4):
                S.op('act', lambda e, d_=d_, h=h: e.activation(out=Z[:, d_ * 4 + h:d_ * 4 + h + 1], in_=lg[:, d_ * 4 + h:d_ * 4 + h + 1], func=AF.Exp, scale=pcol(d_)),
                     r=lg.all() + rc.all(), w=Z.all())
        S.op('act', lambda e: e.activation(out=G128[:], in_=lg[:], func=AF.Exp, scale=128.0), r=lg.all(), w=G128.all())
        Xi = sb('Xi', [128, 8, 128], F32)
        DfT = sb('DfT', [128, 4, 128], F32)
        dtmp = sb('dtmp', [128, 128], F32)
        for d_ in range(2):
            for h in range(4):
                S.op('act', lambda e, d_=d_, h=h: e.activation(out=Xi[:, d_ * 4 + h, :], in_=tab(d_), func=AF.Exp, scale=lg[:, d_ * 4 + h:d_ * 4 + h + 1]),
                     r=lg.all() + rc.all(), w=Xi.all())
        for h in range(4):
            S.op('act', lambda e, h=h: e.activation(out=DfT[:, h, :], in_=tab(2), func=AF.Exp, scale=lg[:, h:h + 1]), r=lg.all() + rc.all(), w=DfT.all())
            S.op('dve', lambda e, h=h: e.tensor_tensor(out=DfT[:, h, :], in0=DfT[:, h, :], in1=tab(3), op=ALU.mult), r=DfT.all() + rc.all(), w=DfT.all())
            S.op('act', lambda e, h=h: e.activation(out=dtmp[:], in_=tab(4), func=AF.Exp, scale=lg[:, 4 + h:5 + h]), r=lg.all() + rc.all(), w=dtmp.all())
            S.op('dve', lambda e, h=h: e.tensor_tensor(out=dtmp[:], in0=dtmp[:], in1=tab(5), op=ALU.mult), r=dtmp.all() + rc.all(), w=dtmp.all())
            S.op('dve', lambda e, h=h: e.tensor_tensor(out=DfT[:, h, :], in0=DfT[:, h, :], in1=dtmp[:], op=ALU.add), r=DfT.all() + dtmp.all(), w=DfT.all())
        cE = sb('cE', [128, 2 * (NR + 1)], F32)
        cM = sb('cM', [128, 2 * (NR + 1)], F32)
        load_bc(S, g, cE, g.coefE[0:1, :])
        load_bc(S, g, cM, g.coefM[0:1, :])
        coef = sb('coef', [128, 8, NR + 1], F32)
        for d_ in range(2):
            for h in range(4):
                S.op('act', lambda e, d_=d_, h=h: e.activation(out=coef[:, d_ * 4 + h, :], in_=cE[:, d_ * (NR + 1):(d_ + 1) * (NR + 1)], func=AF.Exp,
                                                               scale=lg[:, d_ * 4 + h:d_ * 4 + h + 1]), r=lg.all() + cE.all(), w=coef.all())
                S.op('dve', lambda e, d_=d_, h=h: e.tensor_tensor(out=coef[:, d_ * 4 + h, :], in0=coef[:, d_ * 4 + h, :], in1=cM[:, d_ * (NR + 1):(d_ + 1) * (NR + 1)], op=ALU.mult),
                     r=coef.all() + cM.all(), w=coef.all())
        gnb = sb('gnb', [128, 512], F32)
        load_bc(S, g, gnb, g.gn[0:1, :])
        c0 = lay['q_r']
        WA, WAp, WB_ = (sb(n, [128, KD, 512], BF16) for n in ('WA', 'WAp', 'WB_'))

        def load_pair(W, Wp, Wo, nm, nmo):
            load_w(S, g, W, g.w_in[:, lay[nm]:lay[nm] + 512], KD)
            load_w(S, g, Wo, g.w_in[:, lay[nmo]:lay[nmo] + 512], KD)
            for h in range(4):
                for half in range(2):
                    src0 = lay[nm] + h * 128 + (1 - half) * 64
                    S.op('pool', lambda e, h=h, half=half, src0=src0: e.dma_start(out=Wp[:, :, h * 128 + half * 64:h * 128 + half * 64 + 64],
                                                                                 in_=g.w_in[:, src0:src0 + 64].rearrange("(c p) n -> p c n", p=128)),
                         w=Wp.all(), dma=True)
        load_pair(WA, WAp, WB_, 'k_r', 'v_r')
        Wk, Wkp, Wv = WA, WAp, WB_
        Wq, Wqp, Wgr = WA, WAp, WB_
        ropc = sb('ropc', [128, 2, 128], F32)
        kT = sb('kT', [128, 4, TCH * 128], BF16, TCH)
        vS = sb('vS', [128, TCH, 512], BF16, TCH)
        rt = sb('rt', [128, 512], F32)
        pA, pB = g.psb[0], g.psb[1]

        def rope_proj(W, Wp, ci, dst):
            proj_fm(S, g, pA, W, 4, hT, ci * 128, 128)
            if ci < NCH:
                proj_fm(S, g, pB, Wp, 4, hT, ci * 128, 128)
                S.op('sp', lambda e: e.dma_start(out=ropc[:], in_=g.rope_r[:, :, ci * 128:(ci + 1) * 128].rearrange("a p n -> p a n")), w=ropc.all(), dma=True)
                for h in range(4):
                    S.op('dve', lambda e, h=h: e.tensor_tensor(out=rt[:, h * 128:(h + 1) * 128], in0=pA[:, h * 128:(h + 1) * 128], in1=ropc[:, 0, :], op=ALU.mult),
                         r=pA.all() + ropc.all(), w=rt.all())
                    S.op('dve', lambda e, h=h: e.tensor_tensor(out=dst(h), in0=pB[:, h * 128:(h + 1) * 128], in1=ropc[:, 1, :], op=ALU.mult),
                         r=pB.all() + ropc.all(), w=dst.keys)
                    S.op('pool', lambda e, h=h: e.tensor_tensor(out=dst(h), in0=dst(h), in1=rt[:, h * 128:(h + 1) * 128], op=ALU.add), r=rt.all() + dst.keys, w=dst.keys)
            else:
                for h in range(4):
                    S.op('act', lambda e, h=h: e.activation(out=dst(h), in_=pA[:, h * 128:(h + 1) * 128], func=AF.Identity), r=pA.all(), w=dst.keys)

        for ci in range(TCH):
            dst = lambda h, ci=ci: kT[:, h, ci * 128:(ci + 1) * 128]
            dst.keys = kT.k(ci)
            rope_proj(Wk, Wkp, ci, dst)
            proj_tm(S, g, pA, Wv, 512, hT, ci)
            S.op('act', lambda e, ci=ci: e.activation(out=vS[:, ci, :], in_=pA[:, 0:512], func=AF.Copy, scale=128.0 ** -0.5), r=pA.all(), w=vS.k(ci))

        load_pair(WA, WAp, WB_, 'q_r', 'g_r')
        Wret = sb('Wret', [128, 4, D], BF16)
        Wg = sb('Wg', [128, KD, D], BF16)
        load_w(S, g, Wret, g.w_ret, 4)
        load_w(S, g, Wg, g.w_in[:, lay['gates']:lay['gates'] + D], KD)
        ktok = sb('ktok', [128, 512], BF16)
        vz = sb('vz', [128, 512], BF16)
        pS = g.psb[2]

        def state_step(R, ci, d_):
            for h in range(4):
                S.op('pe', lambda e, h=h: e.transpose(out=g.pst[:, h * 128:(h + 1) * 128], in_=kT[:, h, ci * 128:(ci + 1) * 128], identity=g.ident[:]),
                     r=kT.k(ci) + g.ident.all(), w=g.pst.all())
            S.op('act', lambda e: e.activation(out=ktok[:], in_=g.pst[:, 0:512], func=AF.Identity), r=g.pst.all(), w=ktok.all())
            for h in range(4):
                S.op('dve', lambda e, h=h: e.tensor_scalar(out=vz[:, h * 128:(h + 1) * 128], in0=vS[:, ci, h * 128:(h + 1) * 128], scalar1=Z[:, d_ * 4 + h:d_ * 4 + h + 1],
                                                           scalar2=None, op0=ALU.mult), r=vS.k(ci) + Z.all(), w=vz.all())
            for h in range(4):
                S.op('pe', lambda e, h=h: e.matmul(pS[:, h * 128:(h + 1) * 128], lhsT=ktok[:, h * 128:(h + 1) * 128], rhs=vz[:, h * 128:(h + 1) * 128], start=True, stop=True),
                     r=ktok.all() + vz.all(), w=pS.all())
            for h in range(4):
                S.op('dve', lambda e, h=h: e.scalar_tensor_tensor(out=R[:, h, :], in0=R[:, h, :], scalar=G128[:, d_ * 4 + h:d_ * 4 + h + 1], in1=pS[:, h * 128:(h + 1) * 128],
                                                                  op0=ALU.mult, op1=ALU.add), r=R.all() + G128.all() + pS.all(), w=R.all())

        Rf, Rb = sb('Rf', [128, 4, 128], F32), sb('Rb', [128, 4, 128], F32)
        Cf, Cb = sb('Cf', [128, 4, 128], F32), sb('Cb', [128, 4, 128], F32)
        zero = lambda R: S.op('pool', lambda e: e.memset(R[:], 0.0), w=R.all())
        zero(Cf), zero(Cb)
        for ci in (NCH, NCH + 1):
            state_step(Cf, ci, 0)
        for ci in (NCH + 1, NCH):
            state_step(Cb, ci, 1)
        zero(Rf), zero(Rb)
        for ci in range(NCH):
            state_step(Rf, ci, 0)
        for ci in reversed(range(NCH)):
            state_step(Rb, ci, 1)
        S.op('sp', lambda e: e.dma_start(out=g.shat[0], in_=Rf[:].rearrange("p h e -> p (h e)")), r=Rf.all(), w=[('shat', 0)], dma=True)
        S.op('sp', lambda e: e.dma_start(out=g.shat[1], in_=Rb[:].rearrange("p h e -> p (h e)")), r=Rb.all(), w=[('shat', 1)], dma=True)
        sin_ = sb('sin_', [128, 512], F32)
        for d_, (R, C) in enumerate(((Rf, Cf), (Rb, Cb))):
            for h in range(4):
                S.op('dve', lambda e, h=h, R=R, C=C, d_=d_: e.tensor_scalar(out=R[:, h, :], in0=C[:, h, :], scalar1=coef[:, d_ * 4 + h, NR:NR + 1], scalar2=None, op0=ALU.mult),
                     r=C.all() + coef.all(), w=R.all())
            for i in range(NR):
                S.op('sp', lambda e, d_=d_, i=i: e.dma_start(out=sin_[:], in_=g.sall[d_, i]), w=sin_.all(), dma=True)
                for h in range(4):
                    S.op('dve', lambda e, h=h, R=R, i=i, d_=d_: e.scalar_tensor_tensor(out=R[:, h, :], in0=sin_[:, h * 128:(h + 1) * 128], scalar=coef[:, d_ * 4 + h, i:i + 1],
                                                                                       in1=R[:, h, :], op0=ALU.mult, op1=ALU.add), r=sin_.all() + coef.all() + R.all(), w=R.all())
        g.uid = getattr(g, 'uid', 0) + 1
        RBd = g.nc.dram_tensor('rbs_scratch%d' % g.uid, [TCH, 128, 512], BF16, kind="Internal").ap()
        rbw = sb('rbw', [128, 4, 128], BF16)
        rbc = sb('rbc', [128, 4, 128], BF16)
        Rfb = sb('Rfb', [128, 4, 128], BF16)
        qT, qf, qb = sb('qT', [128, 4, 128], BF16), sb('qf', [128, 4, 128], BF16), sb('qb', [128, 4, 128], BF16)
        ST = sb('ST', [128, 4, 128], BF16)
        yt, gsil = sb('yt', [128, 512], F32), sb('gsil', [128, 512], F32)
        ya = sb('ya', [128, 512], BF16)
        st = sb('st', [128, 24], F32)
        wk = chain_wk(S, es, g)
        junk = wk[1]
        pC, pD = g.psb[6], g.psb[5]

        def full_pass(chunks, R_f, R_b):
            n0 = chunks[0]
            for ci in reversed(chunks):
                S.op('act', lambda e, ci=ci: e.activation(out=rbw[:], in_=R_b[:], func=AF.Identity), r=R_b.all(), w=rbw.all())
                S.op('sp', lambda e, ci=ci: e.dma_start(out=RBd[ci], in_=rbw[:].rearrange("p h e -> p (h e)")), r=rbw.all(), w=[('rbd', ci)], dma=True)
                state_step(R_b, ci, 1)
            for ci in chunks:
                chunk_body(ci, n0, R_f)

        def chunk_body(ci, n0, R_f):
            if True:
                dst = lambda h: qT[:, h, :]
                dst.keys = qT.all()
                rope_proj(Wq, Wqp, ci, dst)
                S.op('act', lambda e: e.activation(out=Rfb[:], in_=R_f[:], func=AF.Identity), r=R_f.all(), w=Rfb.all())
                S.op('sp', lambda e: e.dma_start(out=rbc[:].rearrange("p h e -> p (h e)"), in_=RBd[ci]), r=[('rbd', ci)], w=rbc.all(), dma=True)
                for h in range(4):
                    S.op('pe', lambda e, h=h: e.matmul(pC[:, h * 128:(h + 1) * 128], lhsT=kT[:, h, ci * 128:(ci + 1) * 128], rhs=qT[:, h, :], start=True, stop=True),
                         r=kT.k(ci) + qT.all(), w=pC.all())
                    S.op('dve', lambda e, h=h: e.tensor_tensor(out=ST[:, h, :], in0=pC[:, h * 128:(h + 1) * 128], in1=DfT[:, h, :], op=ALU.mult), r=pC.all() + DfT.all(), w=ST.all())
                    S.op('pool', lambda e, h=h: e.tensor_tensor(out=qf[:, h, :], in0=qT[:, h, :], in1=Xi[:, h, :], op=ALU.mult), r=qT.all() + Xi.all(), w=qf.all())
                    S.op('pool', lambda e, h=h: e.tensor_tensor(out=qb[:, h, :], in0=qT[:, h, :], in1=Xi[:, 4 + h, :], op=ALU.mult), r=qT.all() + Xi.all(), w=qb.all())
                for h in range(4):
                    hs = slice(h * 128, (h + 1) * 128)
                    S.op('pe', lambda e, h=h, hs=hs: e.matmul(pD[:, hs], lhsT=ST[:, h, :], rhs=vS[:, ci, hs], start=True, stop=False), r=ST.all() + vS.k(ci), w=pD.all())
                    S.op('pe', lambda e, h=h, hs=hs: e.matmul(pD[:, hs], lhsT=qf[:, h, :], rhs=Rfb[:, h, :], start=False, stop=False), r=qf.all() + Rfb.all(), w=pD.all())
                    S.op('pe', lambda e, h=h, hs=hs: e.matmul(pD[:, hs], lhsT=qb[:, h, :], rhs=rbc[:, h, :], start=False, stop=True), r=qb.all() + rbc.all(), w=pD.all())
                proj_tm(S, g, pA, Wgr, 512, hT, ci)
                S.op('act', lambda e: e.activation(out=gsil[:], in_=pA[:, 0:512], func=AF.Silu), r=pA.all(), w=gsil.all())
                for h in range(4):
                    hs = slice(h * 128, (h + 1) * 128)
                    S.op('act', lambda e, h=h, hs=hs: e.activation(out=yt[:, hs], in_=pD[:, hs], func=AF.Identity, accum_out=st[:, h:h + 1]), r=pD.all(), w=yt.all() + st.all())
                    S.op('act', lambda e, h=h, hs=hs: e.activation(out=junk[:, hs], in_=yt[:, hs], func=AF.Square, accum_out=st[:, 4 + h:5 + h]), r=yt.all(), w=junk.all() + st.all())
                S.op('dve', lambda e: e.tensor_scalar(out=st[:, 8:12], in0=st[:, 0:4], scalar1=1.0 / 128, scalar2=None, op0=ALU.mult), r=st.all(), w=st.all())
                S.op('dve', lambda e: e.tensor_tensor(out=st[:, 12:16], in0=st[:, 8:12], in1=st[:, 8:12], op=ALU.mult), r=st.all(), w=st.all())
                S.op('dve', lambda e: e.scalar_tensor_tensor(out=st[:, 16:20], in0=st[:, 4:8], scalar=1.0 / 128, in1=st[:, 12:16], op0=ALU.mult, op1=ALU.subtract), r=st.all(), w=st.all())
                S.op('dve', lambda e: e.tensor_scalar(out=st[:, 16:20], in0=st[:, 16:20], scalar1=1e-6, scalar2=None, op0=ALU.add), r=st.all(), w=st.all())
                S.op('act', lambda e: e.activation(out=st[:, 16:20], in_=st[:, 16:20], func=AF.Sqrt), r=st.all(), w=st.all())
                S.op('dve', lambda e: e.reciprocal(out=st[:, 20:24], in_=st[:, 16:20]), r=st.all(), w=st.all())
                for h in range(4):
                    hs = slice(h * 128, (h + 1) * 128)
                    S.op('dve', lambda e, h=h, hs=hs: e.tensor_scalar(out=yt[:, hs], in0=yt[:, hs], scalar1=st[:, 8 + h:9 + h], scalar2=st[:, 20 + h:21 + h], op0=ALU.subtract, op1=ALU.mult),
                         r=yt.all() + st.all(), w=yt.all())
                S.op('dve', lambda e: e.tensor_tensor(out=yt[:], in0=yt[:], in1=gnb[:], op=ALU.mult), r=yt.all() + gnb.all(), w=yt.all())
                S.op('dve', lambda e: e.tensor_tensor(out=ya[:], in0=yt[:], in1=gsil[:], op=ALU.mult), r=yt.all() + gsil.all(), w=ya.all())
                state_step(R_f, ci, 0)
                out_chain(S, g, ya, Wret, Wg, hT, ci, wk)

        full_pass(list(range(NCH)), Rf, Rb)
        if need_ctx:
            zero(Cf), zero(Cb)
            full_pass([NCH, NCH + 1], Cf, Cb)


def attn_unit(S, g, qT, rows, t, blocks, KT, VA, vsel, esc, den_add, ydst, ykeys, wkA):
    PT, dn = wkA
    pS, pO = g.psb[6], g.psb[5]
    nb = len(blocks)
    for bi, (j, kkeys, vkeys, extra) in enumerate(blocks):
        sl = slice((bi % 4) * 128, (bi % 4 + 1) * 128)
        S.op('pe', lambda e, j=j, sl=sl, extra=extra: e.matmul(pS[:, sl], lhsT=KT(j), rhs=qT[rows, t, :], start=True, stop=(len(extra) == 0)),
             r=kkeys + qT.all(), w=pS.all())
        for xi, (la, ra, rk) in enumerate(extra):
            S.op('pe', lambda e, sl=sl, la=la, ra=ra, xi=xi, extra=extra: e.matmul(pS[:, sl], lhsT=la, rhs=ra, start=False, stop=(xi == len(extra) - 1)), r=rk, w=pS.all())
        S.op('act', lambda e, sl=sl, bi=bi: e.activation(out=PT[:, bi % 4, :], in_=pS[:, sl], func=AF.Exp, scale=esc), r=pS.all(), w=PT.k(bi % 4))
        S.op('pe', lambda e, j=j, bi=bi: e.matmul(pO[:, 0:65], lhsT=PT[:, bi % 4, :], rhs=VA(j), start=(bi == 0), stop=(bi == nb - 1)), r=PT.k(bi % 4) + vkeys, w=pO.all())
    if den_add is not None:
        S.op('dve', lambda e: e.tensor_scalar(out=dn[:, 0:1], in0=pO[:, 64:65], scalar1=den_add, scalar2=None, op0=ALU.add), r=pO.all() + g.esink.all(), w=dn.all())
    else:
        S.op('dve', lambda e: e.tensor_copy(out=dn[:, 0:1], in_=pO[:, 64:65]), r=pO.all(), w=dn.all())
    S.op('dve', lambda e: e.reciprocal(out=dn[:, 1:2], in_=dn[:, 0:1]), r=dn.all(), w=dn.all())
    S.op('dve', lambda e: e.tensor_scalar(out=ydst, in0=pO[:, 0:64], scalar1=dn[:, 1:2], scalar2=None, op0=ALU.mult), r=pO.all() + dn.all(), w=ykeys)


def window(S, g, hT, need_ctx):
    D, KD, NCH, NR = g.D, g.KD, g.NCH, g.NR
    TCH = NCH + 2
    lay = g.lay
    NS = NCH + 4
    stor = lambda ci: ci + 1 if ci < NCH else ci + 2
    with S.scope() as es:
        sb = lambda n, sh, dt, ns=1: S.sbs(es, n, sh, dt, ns)
        Wq, Wqp = sb('Wq', [128, KD, 512], BF16), sb('Wqp', [128, KD, 512], BF16)
        Wk, Wkp, Wv = sb('Wk', [128, KD, 128], BF16), sb('Wkp', [128, KD, 128], BF16), sb('Wv', [128, KD, 128], BF16)
        wsrc = lambda c0, n: g.w_in[:, c0:c0 + n].rearrange("(c p) n -> p c n", p=128)
        for t in range(4):
            for u in range(2):
                hd = t + 4 * u
                c0 = lay['q_w'] + hd * 64
                S.op('pool', lambda e, t=t, u=u, c0=c0: e.dma_start(out=Wq[:, :, t * 128 + u * 64:t * 128 + u * 64 + 64], in_=wsrc(c0, 64)), w=Wq.all(), dma=True)
                for half in range(2):
                    S.op('pool', lambda e, t=t, u=u, c0=c0, half=half: e.dma_start(out=Wqp[:, :, t * 128 + u * 64 + half * 32:t * 128 + u * 64 + half * 32 + 32],
                                                                                   in_=wsrc(c0 + (1 - half) * 32, 32)), w=Wqp.all(), dma=True)
        load_w(S, g, Wk, g.w_in[:, lay['k_w']:lay['k_w'] + 128], KD)
        load_w(S, g, Wv, g.w_in[:, lay['v_w']:lay['v_w'] + 128], KD)
        for u in range(2):
            for half in range(2):
                c0 = lay['k_w'] + u * 64
                S.op('pool', lambda e, u=u, c0=c0, half=half: e.dma_start(out=Wkp[:, :, u * 64 + half * 32:u * 64 + half * 32 + 32], in_=wsrc(c0 + (1 - half) * 32, 32)), w=Wkp.all(), dma=True)
        Wwin, Wg = sb('Wwin', [128, 4, D], BF16), sb('Wg', [128, KD, D], BF16)
        load_w(S, g, Wwin, g.w_win, 4)
        load_w(S, g, Wg, g.w_in[:, lay['gates'] + D:lay['gates'] + 2 * D], KD)
        ropc = sb('ropcw', [128, 2, 128], F32)
        WM = sb('WM', [128, 4, 128], BF16)
        S.op('pool', lambda e: e.dma_start(out=WM[:], in_=g.wmask.rearrange("p (a q) -> p a q", a=4)), w=WM.all(), dma=True)
        g.esink = sb('esink', [128, 8], F32)
        load_bc(S, g, g.esink, g.sink[0:1, :])
        S.op('act', lambda e: e.activation(out=g.esink[:], in_=g.esink[:], func=AF.Exp), r=g.esink.all(), w=g.esink.all())
        kwT = sb('kwT', [128, NS * 128], BF16, NS)
        vwA = sb('vwA', [128, NS, 2, 65], BF16, NS)
        S.op('pool', lambda e: e.memset(vwA[:], 1.0), w=vwA.all())
        rt = sb('rt', [128, 512], F32)
        pA, pB = g.psb[0], g.psb[1]

        def rope_fm(W, Wp, ntile, ci, dst, dkeys):
            proj_fm(S, g, pA, W, ntile, hT, ci * 128, 128)
            if ci < NCH:
                proj_fm(S, g, pB, Wp, ntile, hT, ci * 128, 128)
                S.op('sp', lambda e: e.dma_start(out=ropc[:], in_=g.rope_w[:, :, ci * 128:(ci + 1) * 128].rearrange("a p n -> p a n")), w=ropc.all(), dma=True)
                for t in range(ntile):
                    ts_ = slice(t * 128, (t + 1) * 128)
                    S.op('dve', lambda e, ts_=ts_: e.tensor_tensor(out=rt[:, ts_], in0=pA[:, ts_], in1=ropc[:, 0, :], op=ALU.mult), r=pA.all() + ropc.all(), w=rt.all())
                    S.op('dve', lambda e, ts_=ts_, t=t: e.tensor_tensor(out=dst(t), in0=pB[:, ts_], in1=ropc[:, 1, :], op=ALU.mult), r=pB.all() + ropc.all(), w=dkeys)
                    S.op('pool', lambda e, ts_=ts_, t=t: e.tensor_tensor(out=dst(t), in0=dst(t), in1=rt[:, ts_], op=ALU.add), r=rt.all() + dkeys, w=dkeys)
            else:
                for t in range(ntile):
                    S.op('act', lambda e, t=t: e.activation(out=dst(t), in_=pA[:, t * 128:(t + 1) * 128], func=AF.Identity), r=pA.all(), w=dkeys)

        def kv_chunk(ci):
            j = stor(ci)
            rope_fm(Wk, Wkp, 1, ci, lambda t: kwT[:, j * 128:(j + 1) * 128], kwT.k(j))
            proj_tm(S, g, pA, Wv, 128, hT, ci)
            S.op('act', lambda e: e.activation(out=vwA[:, j, :, 0:64], in_=pA[:, 0:128].rearrange("p (u d) -> p u d", u=2), func=AF.Identity), r=pA.all(), w=vwA.k(j))
        for ci in range(TCH):
            kv_chunk(ci)
        for side, j in ((0, 0), (1, NCH + 1)):
            S.op('pool', lambda e, side=side, j=j: e.dma_start(out=kwT[:, j * 128:(j + 1) * 128], in_=g.kw_h[:, side * 128:(side + 1) * 128]), w=kwT.k(j), dma=True)
            S.op('pool', lambda e, side=side, j=j: e.dma_start(out=vwA[:, j, :, 0:64], in_=g.vw_h[side * 128:(side + 1) * 128, :].rearrange("p (u d) -> p u d", u=2)), w=vwA.k(j), dma=True)
        for side, j in ((0, 1), (1, NCH)):
            S.op('pool', lambda e, side=side, j=j: e.dma_start(out=g.kw_e[:, side * 128:(side + 1) * 128], in_=kwT[:, j * 128:(j + 1) * 128]), r=kwT.k(j), w=[('kw_e', 0)], dma=True)
            S.op('pool', lambda e, side=side, j=j: e.dma_start(out=g.vw_e[side * 128:(side + 1) * 128, :].rearrange("p (u d) -> p u d", u=2), in_=vwA[:, j, :, 0:64]), r=vwA.k(j), w=[('vw_e', 0)], dma=True)
        qT = sb('qT', [128, 4, 128], BF16)
        yb = sb('yb', [128, 512], BF16)
        wkA = (sb('PT', [128, 4, 128], BF16, 4), sb('dn', [128, 2], F32))
        wk = chain_wk(S, es, g)

        def q_chunk(ci):
            rope_fm(Wq, Wqp, 4, ci, lambda t: qT[:, t, :], qT.all())
            for t in range(4):
                for u in range(2):
                    hd = t + 4 * u
                    rows = slice(u * 64, (u + 1) * 64)
                    if ci < NCH:
                        bl = [(ci, 2 if ci == 0 else 0), (ci + 1, None), (ci + 2, 3 if ci == NCH - 1 else 1), (NCH + 2, None), (NCH + 3, None)]
                    else:
                        bl = [(NCH + 2, None), (NCH + 3, None)]
                    blocks = [(j, kwT.k(j), vwA.k(j), ([] if mi is None else [(g.ident[:], WM[:, mi, :], g.ident.all() + WM.all())])) for j, mi in bl]
                    attn_unit(S, g, qT, rows, t, blocks, lambda j, rows=rows: kwT[rows, j * 128:(j + 1) * 128], lambda j, u=u: vwA[:, j, u, :], None, 0.125,
                              g.esink[:, hd:hd + 1], yb[:, hd * 64:(hd + 1) * 64], yb.all(), wkA)
            out_chain(S, g, yb, Wwin, Wg, hT, ci, wk)
        for ci in range(NCH + (2 if need_ctx else 0)):
            q_chunk(ci)


def nattn(S, g, hT, need_ctx):
    D, KD, NCH, NR = g.D, g.KD, g.NCH, g.NR
    TCH = NCH + 2
    lay = g.lay
    NS = NCH + 8
    stor = lambda ci: ci + 3 if ci < NCH else ci + 6
    with S.scope() as es:
        sb = lambda n, sh, dt, ns=1: S.sbs(es, n, sh, dt, ns)
        Wq, Wk, Wv = sb('Wq', [128, KD, 512], BF16), sb('Wk', [128, KD, 512], BF16), sb('Wv', [128, KD, 512], BF16)
        load_w(S, g, Wq, g.w_in[:, lay['q_n']:lay['q_n'] + 512], KD)
        load_w(S, g, Wk, g.w_in[:, lay['k_n']:lay['k_n'] + 512], KD)
        load_w(S, g, Wv, g.w_in[:, lay['v_n']:lay['v_n'] + 512], KD)
        Wna, Wg = sb('Wna', [128, 4, D], BF16), sb('Wg', [128, KD, D], BF16)
        load_w(S, g, Wna, g.w_na, 4)
        load_w(S, g, Wg, g.w_in[:, lay['gates'] + 2 * D:lay['gates'] + 3 * D], KD)
        TB = sb('TB', [128, 8, 7 * 128], BF16)
        for h in range(8):
            S.op('pool', lambda e, h=h: e.dma_start(out=TB[:, h, :], in_=g.tbT[h]), w=TB.all(), dma=True)
        MR = sb('MR', [2, 5 * 7 * 128], BF16)
        S.op('pool', lambda e: e.dma_start(out=MR[:], in_=g.mrow), w=MR.all(), dma=True)
        IK = sb('IK', [2, 128], BF16)
        S.op('pool', lambda e: e.dma_start(out=IK[:], in_=g.indk), w=IK.all(), dma=True)
        knT = sb('knT', [128, 4, NS * 128], BF16, NS)
        vnA = sb('vnA', [128, NS, 8, 65], BF16, NS)
        S.op('pool', lambda e: e.memset(vnA[:], 1.0), w=vnA.all())
        pA = g.psb[0]

        def kv_chunk(ci):
            j = stor(ci)
            proj_fm(S, g, pA, Wk, 4, hT, ci * 128, 128)
            S.op('act', lambda e: e.activation(out=knT[:, :, j * 128:(j + 1) * 128], in_=pA[:, 0:512].rearrange("p (t n) -> p t n", t=4), func=AF.Identity), r=pA.all(), w=knT.k(j))
            proj_tm(S, g, pA, Wv, 512, hT, ci)
            S.op('act', lambda e: e.activation(out=vnA[:, j, :, 0:64], in_=pA[:, 0:512].rearrange("p (h d) -> p h d", h=8), func=AF.Identity), r=pA.all(), w=vnA.k(j))
        for ci in range(TCH):
            kv_chunk(ci)
        khv = g.kn_h.rearrange("p (t c n) -> p t c n", t=4, c=6)
        kev = g.kn_e.rearrange("p (t c n) -> p t c n", t=4, c=6)
        for c in range(6):
            jh = c if c < 3 else NCH + c
            je = 3 + c if c < 3 else NCH + c - 3
            S.op('pool', lambda e, c=c, jh=jh: e.dma_start(out=knT[:, :, jh * 128:(jh + 1) * 128], in_=khv[:, :, c, :]), w=knT.k(jh), dma=True)
            S.op('pool', lambda e, c=c, jh=jh: e.dma_start(out=vnA[:, jh, :, 0:64], in_=g.vn_h[c * 128:(c + 1) * 128, :].rearrange("p (h d) -> p h d", h=8)), w=vnA.k(jh), dma=True)
            S.op('pool', lambda e, c=c, je=je: e.dma_start(out=kev[:, :, c, :], in_=knT[:, :, je * 128:(je + 1) * 128]), r=knT.k(je), w=[('kn_e', 0)], dma=True)
            S.op('pool', lambda e, c=c, je=je: e.dma_start(out=g.vn_e[c * 128:(c + 1) * 128, :].rearrange("p (h d) -> p h d", h=8), in_=vnA[:, je, :, 0:64]), r=vnA.k(je), w=[('vn_e', 0)], dma=True)
        qT = sb('qT', [128, 4, 128], BF16)
        yc = sb('yc', [128, 512], BF16)
        wkA = (sb('PT', [128, 4, 128], BF16, 4), sb('dn', [128, 2], F32))
        wk = chain_wk(S, es, g)

        def q_chunk(ci):
            proj_fm(S, g, pA, Wq, 4, hT, ci * 128, 128)
            S.op('act', lambda e: e.activation(out=qT[:], in_=pA[:, 0:512].rearrange("p (t n) -> p t n", t=4), func=AF.Copy, scale=0.125), r=pA.all(), w=qT.all())
            ty = 0 if ci == 0 else 1 if ci == 1 else 3 if ci == NCH - 2 else 4 if ci == NCH - 1 else 2
            for h in range(8):
                t, u = divmod(h, 2)
                rows = slice(u * 64, (u + 1) * 64)
                blocks = []
                if ci < NCH:
                    for blk in range(7):
                        j = ci + blk
                        m0 = (ty * 7 + blk) * 128
                        blocks.append((j, knT.k(j), vnA.k(j), [(g.ident[:], TB[:, h, blk * 128:(blk + 1) * 128], g.ident.all() + TB.all()),
                                                                 (IK[:], MR[:, m0:m0 + 128], IK.all() + MR.all())]))
                for j in (NCH + 6, NCH + 7):
                    blocks.append((j, knT.k(j), vnA.k(j), []))
                attn_unit(S, g, qT, rows, t, blocks, lambda j, rows=rows, t=t: knT[rows, t, j * 128:(j + 1) * 128], lambda j, h=h: vnA[:, j, h, :], None, 1.0,
                          None, yc[:, h * 64:(h + 1) * 64], yc.all(), wkA)
            out_chain(S, g, yc, Wna, Wg, hT, ci, wk)
        for ci in range(NCH + (2 if need_ctx else 0)):
            q_chunk(ci)


def router(S, g, hT):
    D, KD, NCH, NE = g.D, g.KD, g.NCH, g.NE
    TCH = NCH + 2
    with S.scope() as es:
        norm_all(S, g, es, g.x1, g.g_ffn[0:1, :], 3, 4, hT, range(TCH), deps=lambda ci: [('x1', ci)])
        Wr = S.sbs(es, 'Wr', [128, KD, NE], BF16)
        load_w(S, g, Wr, g.w_router, KD)
        aff = S.sbs(es, 'aff', [128, NE], F32)
        st = S.sbs(es, 'rst', [128, 4], F32)
        affT = S.sbs(es, 'affT', [NE, TCH * 128], F32, TCH)
        pA, pB = g.psb[0], g.psb[1]
        for ci in range(TCH):
            proj_tm(S, g, pA, Wr, NE, hT, ci)
            S.op('dve', lambda e: e.reduce_max(out=st[:, 0:1], in_=pA[:, 0:NE], axis=AX.X), r=pA.all(), w=st.all())
            S.op('dve', lambda e: e.tensor_scalar(out=st[:, 1:2], in0=st[:, 0:1], scalar1=-1.0, scalar2=None, op0=ALU.mult), r=st.all(), w=st.all())
            S.op('act', lambda e: e.activation(out=aff[:], in_=pA[:, 0:NE], func=AF.Exp, bias=st[:, 1:2], scale=1.0, accum_out=st[:, 2:3]), r=pA.all() + st.all(), w=aff.all() + st.all())
            S.op('dve', lambda e: e.reciprocal(out=st[:, 3:4], in_=st[:, 2:3]), r=st.all(), w=st.all())
            S.op('dve', lambda e: e.tensor_scalar(out=aff[:], in0=aff[:], scalar1=st[:, 3:4], scalar2=None, op0=ALU.mult), r=aff.all() + st.all(), w=aff.all())
            S.op('pe', lambda e: e.transpose(out=pB[0:NE, 0:128], in_=aff[:], identity=g.identf[:]), r=aff.all() + g.identf.all(), w=pB.all())
            S.op('act', lambda e, ci=ci: e.activation(out=affT[:, ci * 128:(ci + 1) * 128], in_=pB[0:NE, 0:128], func=AF.Identity), r=pB.all(), w=affT.k(ci))
        S.op('sp', lambda e: e.dma_start(out=g.affT, in_=affT[:]), r=affT.all(), w=[('affT', 0)], dma=True)


def build_mixer(cfg, need_ctx, mixers=('ret', 'win', 'na'), do_router=True):
    nc = bass.Bass("TRN2", target_bir_lowering=False)
    g = Ctx()
    g.nc = nc
    D = g.D = cfg['D']
    KD = g.KD = D // 128
    NCH = g.NCH = cfg['NCH']
    NR = g.NR = cfg['NR']
    g.NE = cfg['NE']
    TCH = NCH + 2
    g.lay, IC = col_layout(D)
    I = lambda n, sh: setattr(g, n, dt_in(nc, n, sh))
    I('xin', [TCH * 128, D]); I('c2', [2, D]); I('w_mod', [D, 6 * D]); I('b_mod', [1, 6 * D]); I('g_mix', [1, D]); I('g_ffn', [1, D])
    I('w_in', [D, IC]); I('decay', [1, 8]); I('gn', [1, 512]); I('w_ret', [512, D]); I('sink', [1, 8]); I('w_win', [512, D]); I('w_na', [512, D])
    I('w_out', [D, D]); I('w_router', [D, g.NE])
    I('rconst', [128, 2 + 128 * 6]); I('coefE', [1, 2 * (NR + 1)]); I('coefM', [1, 2 * (NR + 1)]); I('sall', [2, NR, 128, 512])
    I('rope_r', [2, 128, NCH * 128]); I('rope_w', [2, 128, NCH * 128])
    I('wmask', [128, 4 * 128]); I('kw_h', [128, 2 * 128]); I('vw_h', [2 * 128, 128])
    I('tbT', [8, 128, 7 * 128]); I('mrow', [2, 5 * 7 * 128]); I('indk', [2, 128]); I('kn_h', [128, 4 * 6 * 128]); I('vn_h', [6 * 128, 512])
    O = lambda n, sh: setattr(g, n, dt_out(nc, n, sh))
    O('x1', [TCH * 128, D]); O('affT', [g.NE, TCH * 128]); O('shat', [2, 128, 512])
    O('kw_e', [128, 2 * 128]); O('vw_e', [2 * 128, 128]); O('kn_e', [128, 4 * 6 * 128]); O('vn_e', [6 * 128, 512])
    with ExitStack() as es:
        S = Sched(nc, es)
        common_setup(S, g)
        setup_c(S, g)
        for ci in range(TCH):
            S.op('sp', lambda e, ci=ci: e.dma_start(out=g.x1[ci * 128:(ci + 1) * 128, :], in_=g.xin[ci * 128:(ci + 1) * 128, :]), w=[('x1', ci)], dma=True)
        hT = S.sb('hT', [128, KD, TCH * 128], BF16, TCH)
        norm_all(S, g, es, g.xin, g.g_mix[0:1, :], 0, 1, hT, range(TCH))
        g.mod2 = [S.sb('mod2_%d' % s, [128, D], F32) for s in range(2)]
        for s in range(2):
            mod_bc(S, g, es, 2, s, g.mod2[s])
        g.Wout = S.sb('Wout', [128, KD, D], BF16)
        load_w(S, g, g.Wout, g.w_out, KD)
        if 'ret' in mixers:
            retention(S, g, hT, need_ctx)
        if 'win' in mixers:
            window(S, g, hT, need_ctx)
        if 'na' in mixers:
            nattn(S, g, hT, need_ctx)
        if do_router:
            router(S, g, hT)
        S.finish('sp', [('x1', ci) for ci in range(TCH)] + [('affT', 0), ('shat', 0), ('shat', 1), ('kw_e', 0), ('vw_e', 0), ('kn_e', 0), ('vn_e', 0)])
        S.finish('pool', [('kw_e', 0), ('vw_e', 0), ('kn_e', 0), ('vn_e', 0)])
        S.emit()
    return nc


def rconst_np():
    p = np.arange(128, dtype=np.float32)
    i = np.arange(128, dtype=np.float32)
    j = p[:, None]
    rc = np.zeros((128, 2 + 128 * 6), np.float32)
    rc[:, 0] = 127 - p
    rc[:, 1] = p
    rc[:, 2:130] = (i + 1)[None, :]
    rc[:, 130:258] = (128 - i)[None, :]
    dif = i[None, :] - j
    rc[:, 258:386] = np.maximum(dif, 0)
    rc[:, 386:514] = (dif >= 0)
    rc[:, 514:642] = np.maximum(-dif, 0)
    rc[:, 642:770] = (dif < 0)
    return rc


def rope_np(tok0, ntok, d, scale=1.0):
    t = np.arange(tok0, tok0 + ntok)
    row = (t // 64).astype(np.float32)
    col = (t % 64).astype(np.float32)
    nf = d // 4
    inv = (10000.0 ** (-np.arange(nf, dtype=np.float32) / nf)).astype(np.float32)
    ang = np.concatenate([row[:, None] * inv, col[:, None] * inv], axis=-1).astype(np.float32)
    cos, sin = np.cos(ang), np.sin(ang)
    out = np.zeros((2, 128, ntok), np.float32)
    for p in range(128):
        dl = p % d
        f = dl % (d // 2)
        out[0, p] = cos[:, f] * scale
        out[1, p] = (sin[:, f] if dl >= d // 2 else -sin[:, f]) * scale
    return out


def coef_np(r, NR, NCH):
    L = NCH * 128
    E = np.zeros((2, NR + 1), np.float32)
    M = np.zeros((2, NR + 1), np.float32)
    for i in range(NR):
        if i < r:
            E[0, i], M[0, i] = L * (r - 1 - i), 1
        if i > r:
            E[1, i], M[1, i] = L * (i - r - 1), 1
    E[0, NR], M[0, NR] = L * r, 1
    E[1, NR], M[1, NR] = L * (NR - 1 - r), 1
    return E.reshape(1, -1), M.reshape(1, -1)


def host_tables(cfg, r, rpb):
    NCH, NR = cfg['NCH'], cfg['NR']
    rows = NCH * NR * 2
    k = np.arange(128)
    q = np.arange(128)
    kr, kc = k // 64, k % 64
    qr, c = q // 64, q % 64
    col_start = np.clip(c - 8, 0, 48)
    colv = (kc[:, None] >= col_start[None, :]) & (kc[:, None] < col_start[None, :] + 16)
    cidx = np.clip(kc[:, None] - c[None, :], -15, 15) + 15
    tbT = np.full((8, 128, 7, 128), NEG, np.float32)
    for blk in range(7):
        dl = 2 * (blk - 3) + kr[:, None] - qr[None, :]
        ok = colv & (dl + 7 >= 0) & (dl + 7 <= 14)
        ridx = np.clip(dl + 7, 0, 14)
        for h in range(8):
            tbT[h, :, blk, :] = np.where(ok, rpb[h][ridx, cidx], np.float32(NEG))
    mrow = np.full((2, 5, 7, 128), NEG, np.float32)
    for ty, ci in enumerate((0, 1, 2, NCH - 2, NCH - 1)):
        gc = r * NCH + ci
        for blk in range(7):
            kch = gc + blk - 3
            for a in range(2):
                krow = 2 * kch + a
                qrow = 2 * gc + qr
                st = np.clip(qrow - 4, 0, rows - 8)
                ok = (krow >= st) & (krow < st + 8) & (kch >= 0) & (kch < NCH * NR)
                mrow[a, ty, blk, :] = np.where(ok, 0.0, NEG)
    indk = np.stack([(kr == 0), (kr == 1)]).astype(np.float32)
    wm = np.zeros((128, 4, 128), np.float32)
    wm[:, 0, :] = np.where(k[:, None] >= q[None, :], 0.0, NEG)
    wm[:, 1, :] = np.where(k[:, None] <= q[None, :], 0.0, NEG)
    wm[:, 2, :] = NEG if r == 0 else wm[:, 0, :]
    wm[:, 3, :] = NEG if r == NR - 1 else wm[:, 1, :]
    return dict(tbT=tbT.reshape(8, 128, 896), mrow=mrow.reshape(2, -1), indk=indk, wmask=wm.reshape(128, 512))


def route(cfg, res):
    NR = cfg['NR']
    sall = np.stack([np.stack([res[i]['shat'][d_] for i in range(NR)]) for d_ in range(2)])
    out = []
    for r in range(NR):
        p, n = max(r - 1, 0), min(r + 1, NR - 1)
        kwp, kwn = res[p]['kw_e'][:, 128:256], res[n]['kw_e'][:, 0:128]
        vwp, vwn = res[p]['vw_e'][128:256], res[n]['vw_e'][0:128]
        knp = res[p]['kn_e'].reshape(128, 4, 6, 128)[:, :, 3:6]
        knn = res[n]['kn_e'].reshape(128, 4, 6, 128)[:, :, 0:3]
        vnp, vnn = res[p]['vn_e'][384:768], res[n]['vn_e'][0:384]
        out.append(dict(sall=sall, kw_h=np.concatenate([kwp, kwn], 1), vw_h=np.concatenate([vwp, vwn], 0),
                        kn_h=np.concatenate([knp, knn], 2).reshape(128, -1), vn_h=np.concatenate([vnp, vnn], 0)))
    return out


def bisect_thr(S, g, es, Aap, Akeys, P, L, K, Gm, nit=36):
    sb = lambda n, sh: S.sbs(es, n, sh, F32)
    lo, hi, mid, cnt, m, nm, t1, t2 = (sb(n, [P, 1]) for n in ('lo', 'hi', 'mid', 'cnt', 'm', 'nm', 't1', 't2'))
    junk = sb('bj', [P, L])
    pT = g.psb[0]
    S.op('dve', lambda e: e.memset(lo[:], 0.0), w=lo.all())
    S.op('dve', lambda e: e.memset(hi[:], 1.0), w=hi.all())
    for it in range(nit):
        S.op('dve', lambda e: e.tensor_tensor(out=mid[:], in0=lo[:], in1=hi[:], op=ALU.add), r=lo.all() + hi.all(), w=mid.all())
        S.op('dve', lambda e: e.tensor_scalar(out=mid[:], in0=mid[:], scalar1=0.5, scalar2=None, op0=ALU.mult), r=mid.all(), w=mid.all())
        S.op('dve', lambda e: e.tensor_scalar(out=junk[:], in0=Aap, scalar1=mid[:, 0:1], scalar2=0.0, op0=ALU.is_ge, op1=ALU.add, accum_out=cnt[:, 0:1]),
             r=Akeys + mid.all(), w=junk.all() + cnt.all())
        if Gm is not None:
            S.op('pe', lambda e: e.matmul(pT[0:P, 0:1], lhsT=Gm[:], rhs=cnt[:], start=True, stop=True), r=Gm.all() + cnt.all(), w=pT.all())
            S.op('dve', lambda e: e.tensor_scalar(out=m[:], in0=pT[0:P, 0:1], scalar1=float(K), scalar2=None, op0=ALU.is_ge), r=pT.all(), w=m.all())
        else:
            S.op('dve', lambda e: e.tensor_scalar(out=m[:], in0=cnt[:], scalar1=float(K), scalar2=None, op0=ALU.is_ge), r=cnt.all(), w=m.all())
        S.op('dve', lambda e: e.tensor_scalar(out=nm[:], in0=m[:], scalar1=-1.0, scalar2=1.0, op0=ALU.mult, op1=ALU.add), r=m.all(), w=nm.all())
        S.op('dve', lambda e: e.tensor_tensor(out=t1[:], in0=mid[:], in1=m[:], op=ALU.mult), r=mid.all() + m.all(), w=t1.all())
        S.op('dve', lambda e: e.scalar_tensor_tensor(out=lo[:], in0=lo[:], scalar=nm[:, 0:1], in1=t1[:], op0=ALU.mult, op1=ALU.add), r=lo.all() + nm.all() + t1.all(), w=lo.all())
        S.op('dve', lambda e: e.tensor_tensor(out=t2[:], in0=mid[:], in1=nm[:], op=ALU.mult), r=mid.all() + nm.all(), w=t2.all())
        S.op('dve', lambda e: e.scalar_tensor_tensor(out=hi[:], in0=hi[:], scalar=m[:, 0:1], in1=t2[:], op0=ALU.mult, op1=ALU.add), r=hi.all() + m.all() + t2.all(), w=hi.all())
    return lo


def build_moe(cfg, need_ctx, final):
    nc = bass.Bass("TRN2", target_bir_lowering=False)
    g = Ctx()
    g.nc = nc
    D = g.D = cfg['D']
    KD = g.KD = D // 128
    NCH = g.NCH = cfg['NCH']
    NR = g.NR = cfg['NR']
    NE = g.NE = cfg['NE']
    EH = cfg['EH']
    KE = EH // 128
    TCH = NCH + 2
    NTC = TCH if need_ctx else NCH
    L = NCH * 128
    P = NR * NE
    I = lambda n, sh: setattr(g, n, dt_in(nc, n, sh))
    I('x1', [TCH * 128, D]); I('c2', [2, D]); I('w_mod', [D, 6 * D]); I('b_mod', [1, 6 * D]); I('g_ffn', [1, D]); I('g_final', [1, D])
    I('affl', [NE, TCH * 128]); I('affall', [P, L]); I('gmat', [P, P])
    I('wg', [NE, D, EH]); I('wu', [NE, D, EH]); I('wd', [NE, EH, D])
    g.x2 = dt_out(nc, 'x2', [NTC * 128, D])
    with ExitStack() as es:
        S = Sched(nc, es)
        common_setup(S, g)
        setup_c(S, g)
        gw = S.sb('gw', [128, TCH, NE], F32, TCH)
        with S.scope() as es2:
            A = S.sbs(es2, 'A', [P, L], F32)
            Gm = S.sbs(es2, 'Gm', [P, P], F32)
            al = S.sbs(es2, 'al', [NE, TCH * 128], F32)
            gT = S.sbs(es2, 'gT', [NE, TCH * 128], F32)
            S.op('sp', lambda e: e.dma_start(out=A[:], in_=g.affall), w=A.all(), dma=True)
            S.op('sp', lambda e: e.dma_start(out=Gm[:], in_=g.gmat), w=Gm.all(), dma=True)
            S.op('sp', lambda e: e.dma_start(out=al[:], in_=g.affl), w=al.all(), dma=True)
            with S.scope() as es3:
                thr = bisect_thr(S, g, es3, A[:], A.all(), P, L, 2 * (NR * L) // NE, Gm)
                S.op('dve', lambda e: e.scalar_tensor_tensor(out=gT[:, 0:L], in0=al[:, 0:L], scalar=thr[0:NE, 0:1], in1=al[:, 0:L], op0=ALU.is_ge, op1=ALU.mult),
                     r=al.all() + thr.all(), w=gT.all())
            if need_ctx:
                with S.scope() as es3:
                    thc = bisect_thr(S, g, es3, al[:, L:L + 256], al.all(), NE, 256, 2 * 256 // NE, None)
                    S.op('dve', lambda e: e.scalar_tensor_tensor(out=gT[:, L:L + 256], in0=al[:, L:L + 256], scalar=thc[0:NE, 0:1], in1=al[:, L:L + 256], op0=ALU.is_ge, op1=ALU.mult),
                         r=al.all() + thc.all(), w=gT.all())
            for ci in range(NTC):
                S.op('pe', lambda e, ci=ci: e.transpose(out=g.psb[1][:, 0:NE], in_=gT[:, ci * 128:(ci + 1) * 128], identity=g.identf[0:NE, 0:NE]), r=gT.all() + g.identf.all(), w=g.psb[1].all())
                S.op('act', lambda e, ci=ci: e.activation(out=gw[:, ci, :], in_=g.psb[1][:, 0:NE], func=AF.Identity), r=g.psb[1].all(), w=gw.k(ci))
        hT = S.sb('hT', [128, KD, TCH * 128], BF16, TCH)
        norm_all(S, g, es, g.x1, g.g_ffn[0:1, :], 3, 4, hT, range(NTC))
        mod5 = [S.sb('mod5_%d' % s_, [128, D], F32) for s_ in range(2 if need_ctx else 1)]
        for s_ in range(2 if need_ctx else 1):
            mod_bc(S, g, es, 5, s_, mod5[s_])
        acc = S.sb('acc', [128, NTC, D], F32, NTC)
        for ci in range(NTC):
            S.op('pool', lambda e, ci=ci: e.memset(acc[:, ci, :], 0.0), w=acc.k(ci))
        WB = [S.sb('WB%d' % i_, [128, KD * EH], BF16) for i_ in range(4)]
        hid = S.sb('hid', [128, KE, 512], BF16)
        sg = S.sb('sg', [128, 512], F32)
        BW = min(512, D)
        NT = NTC * 128

        def expert(e_):
            Wg_, Wu_, Wd_ = WB[(3 * e_) % 4], WB[(3 * e_ + 1) % 4], WB[(3 * e_ + 2) % 4]
            vg = Wg_[:].rearrange("p (k n) -> p k n", k=KD)
            vu = Wu_[:].rearrange("p (k n) -> p k n", k=KD)
            vd = Wd_[:].rearrange("p (k n) -> p k n", k=KE)
            S.op('pool', lambda e: e.dma_start(out=vg, in_=g.wg[e_].rearrange("(c p) n -> p c n", p=128)), w=Wg_.all(), dma=True)
            S.op('pool', lambda e: e.dma_start(out=vu, in_=g.wu[e_].rearrange("(c p) n -> p c n", p=128)), w=Wu_.all(), dma=True)
            S.op('pool', lambda e: e.dma_start(out=vd, in_=g.wd[e_].rearrange("(c p) n -> p c n", p=128)), w=Wd_.all(), dma=True)
            for t0 in range(0, NT, 512):
                tblock(t0, min(512, NT - t0), vg, vu, vd, Wg_, Wu_, Wd_, e_)

        def tblock(t0, nt, vg, vu, vd, Wg_, Wu_, Wd_, e_):
            if True:
                cks = list(range(t0 // 128, (t0 + nt) // 128))
                for f in range(KE):
                    pG, pU = g.psb[(f % 2) * 2], g.psb[(f % 2) * 2 + 1]
                    for kc in range(KD):
                        S.op('pe', lambda e, f=f, kc=kc, pG=pG: e.matmul(pG[:, 0:nt], lhsT=vg[:, kc, f * 128:(f + 1) * 128], rhs=hT[:, kc, t0:t0 + nt], start=(kc == 0), stop=(kc == KD - 1)),
                             r=Wg_.all() + hT.k(*cks), w=pG.all())
                    for kc in range(KD):
                        S.op('pe', lambda e, f=f, kc=kc, pU=pU: e.matmul(pU[:, 0:nt], lhsT=vu[:, kc, f * 128:(f + 1) * 128], rhs=hT[:, kc, t0:t0 + nt], start=(kc == 0), stop=(kc == KD - 1)),
                             r=Wu_.all() + hT.k(*cks), w=pU.all())
                    S.op('act', lambda e, pG=pG: e.activation(out=sg[:, 0:nt], in_=pG[:, 0:nt], func=AF.Silu), r=pG.all(), w=sg.all())
                    S.op('dve', lambda e, f=f, pU=pU: e.tensor_tensor(out=hid[:, f, 0:nt], in0=pU[:, 0:nt], in1=sg[:, 0:nt], op=ALU.mult), r=pU.all() + sg.all(), w=hid.all())
                for ci in cks:
                    c0 = ci * 128 - t0
                    for hb in range(D // BW):
                        pY = g.psb[4 + (hb % 2)]
                        for f in range(KE):
                            S.op('pe', lambda e, f=f, hb=hb, pY=pY, c0=c0: e.matmul(pY[:, 0:BW], lhsT=hid[:, f, c0:c0 + 128], rhs=vd[:, f, hb * BW:(hb + 1) * BW], start=(f == 0), stop=(f == KE - 1)),
                                 r=hid.all() + Wd_.all(), w=pY.all())
                        S.op('dve', lambda e, hb=hb, pY=pY, ci=ci: e.scalar_tensor_tensor(out=acc[:, ci, hb * BW:(hb + 1) * BW], in0=pY[:, 0:BW], scalar=gw[:, ci, e_:e_ + 1],
                                                                                          in1=acc[:, ci, hb * BW:(hb + 1) * BW], op0=ALU.mult, op1=ALU.add),
                             r=pY.all() + gw.k(ci) + acc.k(ci), w=acc.k(ci))
        for e_ in range(NE):
            expert(e_)
        xt = S.sb('xt2', [128, D], F32)
        junk = S.sb('junk2', [128, D], F32)
        st = S.sb('st2', [128, 4], F32)
        gfb = None
        if final:
            gfb = S.sb('gfb', [128, D], F32)
            load_bc(S, g, gfb, g.g_final[0:1, :])

        def resid(ci):
            s_ = 0 if ci < NCH else 1
            S.op('sp', lambda e: e.dma_start(out=xt[:], in_=g.x1[ci * 128:(ci + 1) * 128, :]), w=xt.all(), dma=True)
            S.op('dve', lambda e: e.tensor_tensor(out=junk[:], in0=acc[:, ci, :], in1=mod5[s_][:], op=ALU.mult), r=acc.k(ci) + mod5[s_].all(), w=junk.all())
            S.op('dve', lambda e: e.tensor_tensor(out=xt[:], in0=xt[:], in1=junk[:], op=ALU.add), r=xt.all() + junk.all(), w=xt.all())
            if final:
                S.op('act', lambda e: e.activation(out=junk[:], in_=xt[:], func=AF.Square, accum_out=st[:, 0:1]), r=xt.all(), w=junk.all() + st.all())
                S.op('dve', lambda e: e.tensor_scalar(out=st[:, 1:2], in0=st[:, 0:1], scalar1=1.0 / D, scalar2=1e-6, op0=ALU.mult, op1=ALU.add), r=st.all(), w=st.all())
                S.op('act', lambda e: e.activation(out=st[:, 2:3], in_=st[:, 1:2], func=AF.Sqrt), r=st.all(), w=st.all())
                S.op('dve', lambda e: e.reciprocal(out=st[:, 3:4], in_=st[:, 2:3]), r=st.all(), w=st.all())
                S.op('dve', lambda e: e.scalar_tensor_tensor(out=xt[:], in0=xt[:], scalar=st[:, 3:4], in1=gfb[:], op0=ALU.mult, op1=ALU.mult), r=xt.all() + st.all() + gfb.all(), w=xt.all())
            S.op('sp', lambda e: e.dma_start(out=g.x2[ci * 128:(ci + 1) * 128, :], in_=xt[:]), r=xt.all(), w=[('x2', ci)], dma=True)
        for ci in range(NTC):
            resid(ci)
        S.finish('sp', [('x2', ci) for ci in range(NTC)])
        S.emit()
    return nc


_PROG_CACHE = {}


def _prog(kind, cfg, *a):
    key = (kind, tuple(sorted(cfg.items())), a)
    if key not in _PROG_CACHE:
        _PROG_CACHE[key] = (build_mixer if kind == 'mixer' else build_moe)(cfg, *a)
    return _PROG_CACHE[key]


def run_module(inp, cfg, runner):
    NR, NCH, D, NE = cfg['NR'], cfg['NCH'], cfg['D'], cfg['NE']
    L = NCH * 128
    f32 = lambda a: np.ascontiguousarray(np.asarray(a, dtype=np.float32))
    x = f32(inp['x'])[0]
    ctx = f32(inp['ctx'])[0]
    xs = [np.concatenate([x[r * L:(r + 1) * L], ctx], 0) for r in range(NR)]
    c2 = np.stack([f32(inp['c'])[0], f32(inp['c_ctx'])])
    rc = rconst_np()
    gmat = np.ascontiguousarray((np.arange(NR * NE)[:, None] % NE == np.arange(NR * NE)[None, :] % NE).astype(np.float32))
    z = lambda *s: np.zeros(s, np.float32)
    DEPTH = 2
    out = None
    for l in range(DEPTH):
        need_ctx = l < DEPTH - 1
        W = {k: f32(inp[k][l]) for k in ('w_mod', 'w_in', 'w_ret', 'w_win', 'w_na', 'w_out', 'w_router')}
        V = {k: f32(inp[k][l]).reshape(1, -1) for k in ('b_mod', 'g_mix', 'g_ffn', 'ret_decay_logit', 'ret_gn', 'win_sink')}
        base = []
        for r in range(NR):
            E, M = coef_np(r, NR, NCH)
            d = dict(xin=xs[r], c2=c2, w_mod=W['w_mod'], b_mod=V['b_mod'], g_mix=V['g_mix'], g_ffn=V['g_ffn'], w_in=W['w_in'], decay=V['ret_decay_logit'],
                     gn=V['ret_gn'], w_ret=W['w_ret'], sink=V['win_sink'], w_win=W['w_win'], w_na=W['w_na'], w_out=W['w_out'], w_router=W['w_router'],
                     rconst=rc, coefE=E, coefM=M, rope_r=rope_np(r * L, L, 128), rope_w=rope_np(r * L, L, 64))
            d.update(host_tables(cfg, r, f32(inp['na_rpb'][l])))
            base.append(d)
        zero = dict(sall=z(2, NR, 128, 512), kw_h=z(128, 256), vw_h=z(256, 128), kn_h=z(128, 4 * 6 * 128), vn_h=z(6 * 128, 512))
        ncm = _prog('mixer', cfg, need_ctx)
        res1 = runner(ncm, [dict(b, **zero) for b in base])
        ext = route(cfg, res1)
        res2 = runner(ncm, [dict(b, **e_) for b, e_ in zip(base, ext)])
        affall = np.ascontiguousarray(np.concatenate([res2[r]['affT'][:, :L] for r in range(NR)], 0))
        ncf = _prog('moe', cfg, need_ctx, not need_ctx)
        ins = [dict(x1=res2[r]['x1'], c2=c2, w_mod=W['w_mod'], b_mod=V['b_mod'], g_ffn=V['g_ffn'], g_final=f32(inp['g_final']).reshape(1, -1),
                    affl=res2[r]['affT'], affall=affall, gmat=gmat, wg=f32(inp['w_exp_gate'][l]), wu=f32(inp['w_exp_up'][l]), wd=f32(inp['w_exp_down'][l]))
               for r in range(NR)]
        res3 = runner(ncf, ins)
        if need_ctx:
            xs = [np.ascontiguousarray(res3[r]['x2']) for r in range(NR)]
        else:
            out = np.concatenate([res3[r]['x2'][:L] for r in range(NR)], 0)[None]
    return np.ascontiguousarray(out.astype(np.float32))


def kernel(**inputs):
    cfg = dict(CFG)
    runner = lambda nc, ins: run_bass_kernel_spmd(nc, ins, core_ids=list(range(cfg['NR']))).results
    return run_module(inputs, cfg, runner)
```
